# Optimizing a Trainium2 kernel written in Bass

```python
import math
import jax
import jax.numpy as jnp
from jax import lax
import numpy as np

D_MODEL = 1024
BATCH = 8
SEQ = 4096
DEPTH = 2

GRID_W = 64
CTX_LEN = 256

N_HEADS_ATTN = 4
HEAD_DIM_QK = 64
HEAD_DIM_V = 2 * HEAD_DIM_QK
QK_W = N_HEADS_ATTN * 2 * HEAD_DIM_QK
ATTN_W = N_HEADS_ATTN * HEAD_DIM_V
ROPE_BASE = 10000.0
ROPE_FREQS = HEAD_DIM_QK // 4
Q_BLOCK = 128

CONV_W = 512
CONV_K = 3

POOL_WINDOWS = (2, 4, 8, 16)
POOL_GROUPS = 4
POOL_W = 512
POOL_GW = POOL_W // POOL_GROUPS

N_BRANCH = 3
Q_END = QK_W
K_END = Q_END + QK_W
V_END = K_END + ATTN_W
CB_END = V_END + CONV_W
CC_END = CB_END + CONV_W
CX_END = CC_END + CONV_W
P_END = CX_END + POOL_W
IN_W = P_END + N_BRANCH * D_MODEL

D_FF = 2816
N_EXPERTS = 8
TOP_K = 2
N_DENSE = (DEPTH + 1) // 2
N_MOE = DEPTH // 2

EPS = 1e-6

kernel_name = "hybrid_diffattn_conv_pool_moe_dit"


def rmsnorm(t, g):
    tf = t.astype(jnp.float32)
    n = tf * lax.rsqrt(jnp.mean(tf * tf, axis=-1, keepdims=True) + EPS)
    return (n * g.astype(jnp.float32)).astype(t.dtype)


def modulate(t, shift, scale):
    return t * (1 + scale) + shift


def rope_tables(rows, cols):
    inv = ROPE_BASE ** (-jnp.arange(ROPE_FREQS, dtype=jnp.float32) / ROPE_FREQS)
    ang = jnp.stack([rows.astype(jnp.float32)[:, None] * inv,
                     cols.astype(jnp.float32)[:, None] * inv], axis=1)
    ang = jnp.broadcast_to(ang[:, :, None, :], (ang.shape[0], 2, 2, ROPE_FREQS))
    ang = ang.reshape(ang.shape[0], HEAD_DIM_QK)
    return jnp.cos(ang), jnp.sin(ang)


def apply_rope(t, cos, sin):
    tr = t.reshape(t.shape[:-1] + (2, 2, ROPE_FREQS))
    rot = jnp.concatenate([-tr[..., 1:2, :], tr[..., 0:1, :]], axis=-2).reshape(t.shape)
    c = cos[None, :, None, None, :]
    s = sin[None, :, None, None, :]
    return (t * c + rot * s).astype(t.dtype)


def heads_qk(t):
    return t.reshape(t.shape[:-1] + (N_HEADS_ATTN, 2, HEAD_DIM_QK))


def heads_v(t):
    return t.reshape(t.shape[:-1] + (N_HEADS_ATTN, HEAD_DIM_V))


def diff_attention(q, k, v, lam):
    B, L = q.shape[0], q.shape[1]
    nblk = L // Q_BLOCK
    qb = jnp.moveaxis(q.reshape((B, nblk, Q_BLOCK) + q.shape[2:]), 1, 0)
    scale = HEAD_DIM_QK ** -0.5

    def block(qi):
        s = jnp.einsum('bqhmd,bkhmd->bhmqk', qi, k).astype(jnp.float32) * scale
        p = jax.nn.softmax(s, axis=-1)
        a = p[:, :, 0] - lam * p[:, :, 1]
        return jnp.einsum('bhqk,bkhe->bqhe', a.astype(v.dtype), v)

    o = lax.map(block, qb)
    return jnp.moveaxis(o, 0, 1).reshape(B, L, N_HEADS_ATTN, HEAD_DIM_V)


def short_conv(u, w):
    p = jnp.pad(u, ((0, 0), (1, 1), (0, 0)))
    return w[0] * p[:, :-2] + w[1] * p[:, 1:-1] + w[2] * p[:, 2:]


def multiscale_pool(u, pool_w, pool_scale):
    B, L = u.shape[0], u.shape[1]
    ug = u.reshape(B, L, POOL_GROUPS, POOL_GW)
    cs = jnp.pad(jnp.cumsum(ug.astype(jnp.float32), axis=1), ((0, 0), (1, 0), (0, 0), (0, 0)))
    t = jnp.arange(L)
    outs = []
    for g, w in enumerate(POOL_WINDOWS):
        lo = jnp.clip(t - w // 2, 0, L)
        hi = jnp.clip(t - w // 2 + w, 0, L)
        csg = cs[:, :, g]
        mean = (csg[:, hi] - csg[:, lo]) / (hi - lo).astype(jnp.float32)[None, :, None]
        outs.append(mean - ug[:, :, g].astype(jnp.float32))
    p = jnp.stack(outs, axis=2).astype(u.dtype)
    y = jnp.einsum('blgc,gcd->blgd', p, pool_w).reshape(B, L, POOL_W)
    return y * pool_scale


def mixer_merge(attn_heads, cb, cc, cx, pin, gts, g_sub, lam_init, conv_w_i, pool_w_i,
                pool_scale_i, w_branch_i, w_out_i):
    B, L = cb.shape[0], cb.shape[1]
    attn_o = (rmsnorm(attn_heads, g_sub) * (1.0 - lam_init)).reshape(B, L, ATTN_W)
    conv_o = cb * short_conv(cc * cx, conv_w_i)
    pool_o = multiscale_pool(pin, pool_w_i, pool_scale_i)
    g = jax.nn.sigmoid(gts.reshape(B, L, N_BRANCH, D_MODEL))
    merged = (g[:, :, 0] * (attn_o @ w_branch_i[0])
              + g[:, :, 1] * (conv_o @ w_branch_i[1])
              + g[:, :, 2] * (pool_o @ w_branch_i[2]))
    return merged @ w_out_i


def swiglu(h, wg, wu, wd):
    return (jax.nn.silu(h @ wg) * (h @ wu)) @ wd


def moe_ffn(h, wr, wg, wu, wd):
    logits = (h @ wr).astype(jnp.float32)
    top_v, top_i = lax.top_k(logits, TOP_K)
    top_p = jax.nn.softmax(top_v, axis=-1)
    combine = jnp.sum(jax.nn.one_hot(top_i, N_EXPERTS, dtype=jnp.float32) * top_p[..., None],
                      axis=-2).astype(h.dtype)
    out = jnp.zeros_like(h)
    for e in range(N_EXPERTS):
        out = out + combine[..., e:e + 1] * swiglu(h, wg[e], wu[e], wd[e])
    return out


def setup_inputs(seed: int = 0) -> dict:
    key = jax.random.key(seed)
    ks = jax.random.split(key, 32)
    f32 = jnp.float32
    D = D_MODEL

    def nrm(k, shape, scale):
        return jax.random.normal(k, shape, f32) * scale

    return {
        "x": nrm(ks[0], (BATCH, SEQ, D), 1.0),
        "c": nrm(ks[1], (BATCH, D), 1.0),
        "ctx": nrm(ks[2], (BATCH, CTX_LEN, D), 1.0),
        "c_ctx": nrm(ks[3], (D,), 1.0),
        "w_mod": nrm(ks[4], (DEPTH, D, 6 * D), 0.5 * D ** -0.5),
        "b_mod": nrm(ks[5], (DEPTH, 6 * D), 0.01),
        "g_pre_mix": 1.0 + nrm(ks[6], (DEPTH, D), 0.05),
        "g_post_mix": 1.0 + nrm(ks[7], (DEPTH, D), 0.05),
        "g_pre_ffn": 1.0 + nrm(ks[8], (DEPTH, D), 0.05),
        "g_post_ffn": 1.0 + nrm(ks[9], (DEPTH, D), 0.05),
        "w_in": nrm(ks[10], (DEPTH, D, IN_W), D ** -0.5),
        "lambda_q1": nrm(ks[11], (DEPTH, HEAD_DIM_QK), 0.1),
        "lambda_k1": nrm(ks[12], (DEPTH, HEAD_DIM_QK), 0.1),
        "lambda_q2": nrm(ks[13], (DEPTH, HEAD_DIM_QK), 0.1),
        "lambda_k2": nrm(ks[14], (DEPTH, HEAD_DIM_QK), 0.1),
        "g_subln": 1.0 + nrm(ks[15], (DEPTH, HEAD_DIM_V), 0.05),
        "conv_w": nrm(ks[16], (DEPTH, CONV_K, CONV_W), CONV_K ** -0.5),
        "pool_w": nrm(ks[17], (DEPTH, POOL_GROUPS, POOL_GW, POOL_GW), POOL_GW ** -0.5),
        "pool_scale": 1.0 + nrm(ks[18], (DEPTH, POOL_W), 0.05),
        "w_branch": nrm(ks[19], (DEPTH, N_BRANCH, ATTN_W, D), ATTN_W ** -0.5),
        "w_out": nrm(ks[20], (DEPTH, D, D), D ** -0.5),
        "ffn_w_gate": nrm(ks[21], (N_DENSE, D, D_FF), D ** -0.5),
        "ffn_w_up": nrm(ks[22], (N_DENSE, D, D_FF), D ** -0.5),
        "ffn_w_down": nrm(ks[23], (N_DENSE, D_FF, D), D_FF ** -0.5),
        "router_w": nrm(ks[24], (N_MOE, D, N_EXPERTS), D ** -0.5),
        "moe_w_gate": nrm(ks[25], (N_MOE, N_EXPERTS, D, D_FF), D ** -0.5),
        "moe_w_up": nrm(ks[26], (N_MOE, N_EXPERTS, D, D_FF), D ** -0.5),
        "moe_w_down": nrm(ks[27], (N_MOE, N_EXPERTS, D_FF, D), D_FF ** -0.5),
    }


def reference(x, c, ctx, c_ctx, w_mod, b_mod, g_pre_mix, g_post_mix, g_pre_ffn, g_post_ffn,
              w_in, lambda_q1, lambda_k1, lambda_q2, lambda_k2, g_subln, conv_w, pool_w,
              pool_scale, w_branch, w_out, ffn_w_gate, ffn_w_up, ffn_w_down, router_w,
              moe_w_gate, moe_w_up, moe_w_down):
    S = x.shape[1]
    ROWS = S // GRID_W
    rows = jnp.repeat(jnp.arange(ROWS), GRID_W)
    cols = jnp.tile(jnp.arange(GRID_W), ROWS)
    cos, sin = rope_tables(rows, cols)
    y = ctx

    for i in range(DEPTH):
        last = i == DEPTH - 1
        mod_lat = (jax.nn.silu(c) @ w_mod[i] + b_mod[i])[:, None, :]
        mod_ctx = jax.nn.silu(c_ctx) @ w_mod[i] + b_mod[i]
        sh_m, sc_m, gt_m, sh_f, sc_f, gt_f = jnp.split(mod_lat, 6, axis=-1)
        csh_m, csc_m, cgt_m, csh_f, csc_f, cgt_f = jnp.split(mod_ctx, 6, axis=-1)

        lam_init = 0.8 - 0.6 * math.exp(-0.3 * i)
        lam = (jnp.exp(jnp.sum(lambda_q1[i].astype(jnp.float32) * lambda_k1[i].astype(jnp.float32)))
               - jnp.exp(jnp.sum(lambda_q2[i].astype(jnp.float32) * lambda_k2[i].astype(jnp.float32)))
               + lam_init)

        h_lat = modulate(rmsnorm(x, g_pre_mix[i]), sh_m, sc_m)
        h_ctx = modulate(rmsnorm(y, g_pre_mix[i]), csh_m, csc_m)

        z = h_lat @ w_in[i]
        q, k, v, cb, cc, cx, pin, gts = jnp.split(
            z, [Q_END, K_END, V_END, CB_END, CC_END, CX_END, P_END], axis=-1)
        q = apply_rope(heads_qk(q), cos, sin)
        k = apply_rope(heads_qk(k), cos, sin)
        v = heads_v(v)

        if last:
            kc, vc = jnp.split(h_ctx @ w_in[i][:, Q_END:V_END], [QK_W], axis=-1)
            kc, vc = heads_qk(kc), heads_v(vc)
        else:
            zc = h_ctx @ w_in[i]
            qc, kc, vc, cbc, ccc, cxc, pinc, gtsc = jnp.split(
                zc, [Q_END, K_END, V_END, CB_END, CC_END, CX_END, P_END], axis=-1)
            qc, kc, vc = heads_qk(qc), heads_qk(kc), heads_v(vc)
            attn_c = diff_attention(qc, kc, vc, lam)
            mix_c = mixer_merge(attn_c, cbc, ccc, cxc, pinc, gtsc, g_subln[i], lam_init, conv_w[i],
                                pool_w[i], pool_scale[i], w_branch[i], w_out[i])

        k_all = jnp.concatenate([kc, k], axis=1)
        v_all = jnp.concatenate([vc, v], axis=1)
        attn_l = diff_attention(q, k_all, v_all, lam)
        mix_l = mixer_merge(attn_l, cb, cc, cx, pin, gts, g_subln[i], lam_init, conv_w[i],
                            pool_w[i], pool_scale[i], w_branch[i], w_out[i])
        x = x + gt_m * rmsnorm(mix_l, g_post_mix[i])
        if not last:
            y = y + cgt_m * rmsnorm(mix_c, g_post_mix[i])

        j = i // 2
        h2 = modulate(rmsnorm(x, g_pre_ffn[i]), sh_f, sc_f)
        if i % 2 == 0:
            f = swiglu(h2, ffn_w_gate[j], ffn_w_up[j], ffn_w_down[j])
        else:
            f = moe_ffn(h2, router_w[j], moe_w_gate[j], moe_w_up[j], moe_w_down[j])
        x = x + gt_f * rmsnorm(f, g_post_ffn[i])
        if not last:
            h2c = modulate(rmsnorm(y, g_pre_ffn[i]), csh_f, csc_f)
            if i % 2 == 0:
                fc = swiglu(h2c, ffn_w_gate[j], ffn_w_up[j], ffn_w_down[j])
            else:
                fc = moe_ffn(h2c, router_w[j], moe_w_gate[j], moe_w_up[j], moe_w_down[j])
            y = y + cgt_f * rmsnorm(fc, g_post_ffn[i])

    return x
```

```python
import math
from contextlib import ExitStack
import numpy as np
import concourse.bass as bass
import concourse.mybir as mybir
from concourse.bass_utils import run_bass_kernel_spmd

F32 = mybir.dt.float32
BF16 = mybir.dt.bfloat16
AF = mybir.ActivationFunctionType
ALU = mybir.AluOpType

D = 1024
T = 4096
TC = 256
NTOK = T + TC
DEPTH = 2
DFF = 2816
NE = 8
EPS = 1e-6
ZW = 4384
ZL = 8
ZC = 4120
CHUNKS = [(c * 512, 512, False) for c in range(8)] + [(T, TC, True)]
TS = 512
NTILE = 23
NSLOT = NTILE * TS
I32 = mybir.dt.int32
import os
SPARSE_MOE = os.environ.get("KDENSE_MOE", "0") != "1"


class Eng:
    def __init__(self, name, eng, sem):
        self.name, self.eng, self.sem = name, eng, sem
        self.seq = 0
        self.sig_seq = []
        self.last = None
        self.last_sig = True
        self.waited = {}


class DSem:
    def __init__(self, sem):
        self.sem = sem
        self.total = 0


class Buf:
    def __init__(self, name, dsem=None):
        self.name = name
        self.dsem = dsem
        self.multi = False
        self.writers = {}
        self.readers = {}


class Sched:
    def __init__(self, nc, stack):
        self.nc = nc
        self.stack = stack
        self.engs = {}
        for nm, e in (("pe", nc.tensor), ("act", nc.scalar), ("dve", nc.vector),
                      ("pool", nc.gpsimd), ("sp", nc.sync)):
            sem = stack.enter_context(nc.semaphore("s_" + nm))
            self.engs[nm] = Eng(nm, e, sem)
        self.dsems = []
        self.dsem_by_name = {}
        self.n_wait = 0
        self.n_inst = 0
        self.dead = False
        self.stop_after = None

    def stop(self, tag):
        if self.stop_after == tag:
            self.dead = True

    def new_dsem(self, name):
        if name in self.dsem_by_name:
            return self.dsem_by_name[name]
        d = DSem(self.stack.enter_context(self.nc.semaphore("d_" + name)))
        self.dsems.append(d)
        self.dsem_by_name[name] = d
        return d

    def buf(self, name, dma=False):
        return Buf(name, name if dma else None)

    def _dsem_for(self, bufs, en):
        for b in bufs:
            if b.dsem is not None:
                return self.new_dsem(b.dsem + "_" + en)
        raise AssertionError("no dma-capable buf")

    def _signal_last(self, E):
        if not E.last_sig:
            E.last.then_inc(E.sem, 1)
            E.last_sig = True
            E.sig_seq.append(E.seq)

    def _resolve(self, tok):
        if tok[0] == "c":
            _, E, seq = tok
            ss = E.sig_seq
            if ss and ss[-1] >= seq:
                lo, hi = 0, len(ss) - 1
                while lo < hi:
                    mid = (lo + hi) // 2
                    if ss[mid] >= seq:
                        hi = mid
                    else:
                        lo = mid + 1
                return E.sem, lo + 1
            assert not E.last_sig and E.seq >= seq
            self._signal_last(E)
            return E.sem, len(ss)
        _, d, val = tok
        return d.sem, d.total

    def _wait(self, E, toks):
        need = {}
        for tok in toks:
            sem, val = self._resolve(tok)
            key = id(sem)
            if E.waited.get(key, 0) >= val:
                continue
            if key not in need or need[key][1] < val:
                need[key] = (sem, val)
        for key, (sem, val) in need.items():
            E.eng.wait_ge(sem, val)
            E.waited[key] = val
            self.n_wait += 1

    def op(self, en, fn, reads=(), writes=(), sig=None):
        if self.dead:
            return None
        E = self.engs[en]
        toks = []
        for b in reads:
            toks.extend(b.writers.values())
        for b in writes:
            for t in list(b.readers.values()) + list(b.writers.values()):
                if t[0] == "c" and t[1] is E:
                    continue
                toks.append(t)
        self._wait(E, toks)
        inst = fn(E.eng)
        E.seq += 1
        E.last = inst
        E.last_sig = False
        self.n_inst += 1
        tok = ("c", E, E.seq)
        for b in reads:
            b.readers[("c", en)] = tok
        for b in writes:
            b.writers = {("c", en): tok}
            b.readers = {}
        if sig or (sig is None and en != "pe"):
            self._signal_last(E)
        return inst

    def dma(self, en, out, in_, reads=(), writes=(), sem_buf=None, **kw):
        if self.dead:
            return None
        E = self.engs[en]
        d = self._dsem_for([sem_buf] if sem_buf is not None else list(writes) + list(reads), en)
        toks = []
        for b in reads:
            toks.extend(b.writers.values())
        for b in writes:
            toks.extend(b.readers.values())
            for t in b.writers.values():
                if t[0] == "d" and (t[1] is d or b.multi):
                    continue
                toks.append(t)
        self._wait(E, toks)
        inst = E.eng.dma_start(out=out, in_=in_, **kw)
        inst.then_inc(d.sem, 16)
        d.total += 16
        self.n_inst += 1
        tok = ("d", d, d.total)
        for b in reads:
            b.readers[("d", id(d))] = tok
        for b in writes:
            if b.multi:
                b.writers = {k: v for k, v in b.writers.items() if k[0] == "d"}
                b.writers[("d", id(d))] = tok
            else:
                b.writers = {("d", id(d)): tok}
            b.readers = {}
        return inst

    def dma_fn(self, en, fn, reads=(), writes=()):
        if self.dead:
            return None
        E = self.engs[en]
        d = self._dsem_for(list(writes) + list(reads), en)
        toks = []
        for b in reads:
            toks.extend(b.writers.values())
        for b in writes:
            toks.extend(b.readers.values())
            for t in b.writers.values():
                if t[0] == "d" and t[1] is d:
                    continue
                toks.append(t)
        self._wait(E, toks)
        inst = fn(E.eng)
        inst.then_inc(d.sem, 16)
        d.total += 16
        self.n_inst += 1
        tok = ("d", d, d.total)
        for b in reads:
            b.readers[("d", id(d))] = tok
        for b in writes:
            b.writers = {("d", id(d)): tok}
            b.readers = {}
        return inst

    def barrier(self):
        if self.dead:
            return
        for E in self.engs.values():
            if E.last is not None:
                self._signal_last(E)
        for E in self.engs.values():
            for E2 in self.engs.values():
                if E2.sig_seq:
                    val = len(E2.sig_seq)
                    if E.waited.get(id(E2.sem), 0) < val:
                        E.eng.wait_ge(E2.sem, val)
                        E.waited[id(E2.sem)] = val
            for d in self.dsems:
                if d.total and E.waited.get(id(d.sem), 0) < d.total:
                    E.eng.wait_ge(d.sem, d.total)
                    E.waited[id(d.sem)] = d.total


def _col(v):
    v = np.asarray(v, np.float32)
    return np.ascontiguousarray(v.reshape(-1, 128).T)


VEC_LAYER = 48 + 8 * 4 + 1 + 12 + 4 + 4
VEC_OFF = {"bmod": 0, "gpm": 48, "gqm": 56, "gpf": 64, "gqf": 72, "gsub": 80, "convw": 81,
           "pscale": 93, "lam": 97}
NV = 16 + DEPTH * VEC_LAYER + 64 + 1
IOTA_COL = NV - 1


def _voff(i, name):
    return 16 + i * VEC_LAYER + VEC_OFF[name]


def _pack_vecs(inp, b):
    v = np.zeros((128, NV), np.float32)
    v[:, 0:8] = _col(inp["c"][b])
    v[:, 8:16] = _col(inp["c_ctx"])
    for i in range(DEPTH):
        v[:, _voff(i, "bmod"):_voff(i, "bmod") + 48] = _col(inp["b_mod"][i])
        v[:, _voff(i, "gpm"):_voff(i, "gpm") + 8] = _col(inp["g_pre_mix"][i])
        v[:, _voff(i, "gqm"):_voff(i, "gqm") + 8] = _col(inp["g_post_mix"][i])
        v[:, _voff(i, "gpf"):_voff(i, "gpf") + 8] = _col(inp["g_pre_ffn"][i])
        v[:, _voff(i, "gqf"):_voff(i, "gqf") + 8] = _col(inp["g_post_ffn"][i])
        v[:, _voff(i, "gsub"):_voff(i, "gsub") + 1] = _col(inp["g_subln"][i])
        for r in range(3):
            v[:, _voff(i, "convw") + r * 4:_voff(i, "convw") + r * 4 + 4] = _col(inp["conv_w"][i][r])
        v[:, _voff(i, "pscale"):_voff(i, "pscale") + 4] = _col(inp["pool_scale"][i])
        for j, nm in enumerate(("lambda_q1", "lambda_k1", "lambda_q2", "lambda_k2")):
            v[0:64, _voff(i, "lam") + j] = np.asarray(inp[nm][i], np.float32)
    ro = 16 + DEPTH * VEC_LAYER
    rw = np.asarray(inp["router_w"][0], np.float32)
    v[:, ro:ro + 64] = rw.reshape(8, 128, 8).transpose(1, 0, 2).reshape(128, 64)
    v[:, IOTA_COL] = np.arange(128, dtype=np.float32)
    return v


def _const_tables():
    rows = np.repeat(np.arange(T // 64), 64).astype(np.float32)
    cols = np.tile(np.arange(64), T // 64).astype(np.float32)
    inv = (10000.0 ** (-np.arange(16, dtype=np.float32) / 16)).astype(np.float32)
    ang = np.stack([rows[:, None] * inv, cols[:, None] * inv], axis=1)
    ang = np.broadcast_to(ang[:, :, None, :], (T, 2, 2, 16)).reshape(T, 64)
    cos = np.cos(ang).astype(np.float32).T
    sin = np.sin(ang).astype(np.float32).T
    sgn = np.ones((64, 1), np.float32)
    pm = np.zeros((128, 128), np.float32)
    for m in range(128):
        dd = m % 64
        half = (dd % 32) // 16
        if half == 0:
            src = m + 16
            sgn[dd, 0] = -1.0
        else:
            src = m - 16
        pm[src, m] = 1.0
    sin = sin * sgn
    cos2 = np.ascontiguousarray(np.concatenate([cos, cos], 0))
    sin2 = np.ascontiguousarray(np.concatenate([sin, sin], 0))
    ident = np.eye(128, dtype=np.float32)
    triu = np.triu(np.ones((128, 128), np.float32), k=1)
    invt = np.ones((3, 4, 512), np.float32)
    for g, w in enumerate((2, 4, 8, 16)):
        def cnt(t, L):
            lo = np.clip(t - w // 2, 0, L)
            hi = np.clip(t - w // 2 + w, 0, L)
            return (hi - lo).astype(np.float32)
        invt[0, g] = 1.0 / cnt(np.arange(0, 512), T)
        invt[1, g] = 1.0 / cnt(np.arange(T - 512, T), T)
        invt[2, g, :TC] = 1.0 / cnt(np.arange(0, TC), TC)
    invt = np.ascontiguousarray(np.broadcast_to(invt[None], (128, 3, 4, 512)))
    return cos2, sin2, pm, ident, invt, triu


WEIGHT_NAMES = ["w_mod", "w_in", "w_branch", "w_out", "pool_w", "ffn_w_gate", "ffn_w_up",
                "ffn_w_down", "moe_w_gate", "moe_w_up", "moe_w_down"]
WEIGHT_SHAPES = {
    "w_mod": [DEPTH, D, 6 * D], "w_in": [DEPTH, D, 6656], "w_branch": [DEPTH, 3, 512, D],
    "w_out": [DEPTH, D, D], "pool_w": [DEPTH, 4, 128, 128], "ffn_w_gate": [1, D, DFF],
    "ffn_w_up": [1, D, DFF], "ffn_w_down": [1, DFF, D], "moe_w_gate": [1, NE, D, DFF],
    "moe_w_up": [1, NE, D, DFF], "moe_w_down": [1, NE, DFF, D],
}


def build_program(n_layers=DEPTH, dbg=False, stop_after=None):
    nc = bass.Bass("TRN2", target_bir_lowering=False)
    din = {}

    def inp(name, shape):
        din[name] = nc.dram_tensor(name, list(shape), F32, kind="ExternalInput").ap()
        return din[name]

    xT_in = inp("xT", [D, T])
    ctxT_in = inp("ctxT", [D, TC])
    vecs_in = inp("vecs", [128, NV])
    cos_in = inp("cos2", [128, T])
    sin_in = inp("sin2", [128, T])
    pm_in = inp("pm", [128, 128])
    ident_in = inp("ident", [128, 128])
    invt_in = inp("invt", [128, 3, 4, 512])
    triu_in = inp("triu", [128, 128])
    if SPARSE_MOE:
        W = {n: inp(n, WEIGHT_SHAPES[n]) for n in WEIGHT_NAMES if not n.startswith("moe_")}
        WgH = inp("moe_w_gate_h", [NE * 2 * D, DFF // 2])
        WuH = inp("moe_w_up_h", [NE * 2 * D, DFF // 2])
        WdF = inp("moe_w_down_f", [NE * DFF, D])
    else:
        W = {n: inp(n, WEIGHT_SHAPES[n]) for n in WEIGHT_NAMES}
    outT = nc.dram_tensor("outT", [D, T], F32, kind="ExternalOutput").ap()

    def scratch(name, shape, dt):
        return nc.dram_tensor(name, list(shape), dt, kind="Internal").ap()

    xA = scratch("xA", [D, T], F32)
    xB = scratch("xB", [D, T], F32)
    yA = scratch("yA", [D, TC], F32)
    yB = scratch("yB", [D, TC], F32)
    ZT = scratch("ZT", [40, 128, ZW], BF16)
    QT = scratch("QT", [4, 128, NTOK], BF16)
    AOT = scratch("AOT", [4, 128, NTOK], BF16)
    H2T = scratch("H2T", [8, 128, NTOK], BF16)
    FT = scratch("FT", [8, 128, NTOK], F32)
    CBd = scratch("CBd", [NE, NTOK], F32)
    XS = scratch("XS", [NSLOT, D], BF16)
    YS = scratch("YS", [NSLOT, D], F32)

    with ExitStack() as st:
        S = Sched(nc, st)
        S.stop_after = stop_after

        uniq = [0]

        def sb(stack, name, shape, dt):
            uniq[0] += 1
            return stack.enter_context(nc.sbuf_tensor(f"sb{uniq[0]}_{name}", list(shape), dt))

        B_xA, B_xB, B_yA, B_yB = (S.buf(n, True) for n in ("xA", "xB", "yA", "yB"))
        B_ZT, B_QT, B_H2T, B_FT, B_CBd, B_out, B_AOT = (S.buf(n, True) for n in ("ZT", "QT", "H2T", "FT", "CBd", "out", "AOT"))
        B_XS, B_YS = S.buf("XS", True), S.buf("YS", True)
        B_ZT.multi = True
        B_QT.multi = True
        B_xin = S.buf("xin")
        B_cin = S.buf("cin")

        vecs = sb(st, "vecs", [128, NV], F32)
        B_vecs = S.buf("vecs", True)
        ones_bf = sb(st, "ones_bf", [128, 128], BF16)
        ones_f = sb(st, "ones_f", [128, 128], F32)
        ident = sb(st, "ident", [128, 128], F32)
        pm = sb(st, "pm", [128, 128], BF16)
        B_const = S.buf("const", True)
        zero_bf = sb(st, "zero_bf", [128, 16], BF16)
        ident_bf = sb(st, "ident_bf", [128, 128], BF16)
        triu = sb(st, "triu", [128, 128], F32)
        mk1s = sb(st, "mk1s", [128, 32, 8], F32)
        mk2s = sb(st, "mk2s", [128, 32, 8], F32)
        rtab = sb(st, "rtab", [128, 4, 32], F32)
        sloti = sb(st, "sloti", [128, 2, 32], I32)
        widx_gu = sb(st, "widx_gu", [128, NTILE, 16], I32)
        widx_d = sb(st, "widx_d", [128, NTILE, 22], I32)
        B_mks, B_rtab, B_sloti, B_widx = S.buf("mks"), S.buf("rtab"), S.buf("sloti"), S.buf("widx")
        silc = sb(st, "silc", [128, 8, 2], BF16)
        B_silc = S.buf("silc")
        mods = []
        for l_ in range(2):
            mods.append((sb(st, f"modT{l_}", [128, 48, 2], F32), sb(st, f"A_m{l_}", [128, 8, 2], F32), sb(st, f"A_f{l_}", [128, 8, 2], F32),
                         sb(st, f"G_m{l_}", [128, 8, 2], F32), sb(st, f"G_f{l_}", [128, 8, 2], F32),
                         sb(st, f"lamv{l_}", [128, 4], F32),
                         S.buf(f"mod{l_}"), S.buf(f"lam{l_}")))
        modT, A_m, A_f, G_m, G_f, lamv, B_mod, B_lam = mods[0]
        ps = [st.enter_context(nc.psum_tensor(f"ps{i}", [128, 512], F32)) for i in range(8)]
        B_ps = [S.buf(f"ps{i}") for i in range(8)]

        S.dma("sp", vecs[:], vecs_in, writes=[B_vecs])
        S.dma("sp", ident[:], ident_in, writes=[B_const])
        S.dma("pool", pm[:], pm_in, writes=[B_const])
        S.dma("pool", ident_bf[:], ident_in, writes=[B_const])
        S.dma("sp", triu[:], triu_in, writes=[B_const])
        S.op("dve", lambda e: e.memset(ones_bf[:], 1.0), writes=[B_const])
        S.op("dve", lambda e: e.memset(ones_f[:], 1.0), writes=[B_const])
        S.op("dve", lambda e: e.memset(zero_bf[:], 0.0), writes=[B_const])
        for j in range(40):
            S.dma("sp", ZT[j, :, 0:8], zero_bf[:, 0:8], reads=[B_const], writes=[B_ZT])
            S.dma("sp", ZT[j, :, ZL + T:ZC], zero_bf[:, 0:16], reads=[B_const], writes=[B_ZT])
            S.dma("sp", ZT[j, :, ZC + TC:ZW], zero_bf[:, 0:8], reads=[B_const], writes=[B_ZT])
        if SPARSE_MOE and n_layers > 1:
            with ExitStack() as zs:
                zrow = sb(zs, "zrow", [128, 1024], BF16)
                B_zrow = S.buf("zrow")
                S.op("pool", lambda e: e.memset(zrow[:], 0.0), writes=[B_zrow])
                for r_ in range(NSLOT // 128):
                    S.dma("sp", XS[r_ * 128:(r_ + 1) * 128, :], zrow[:], reads=[B_zrow], writes=[B_XS])
                S.barrier()
        S.op("act", lambda e: e.activation(out=silc[:, :, 0], in_=vecs[:, 0:8], func=AF.Silu), reads=[B_vecs], writes=[B_silc])
        S.op("act", lambda e: e.activation(out=silc[:, :, 1], in_=vecs[:, 8:16], func=AF.Silu), reads=[B_vecs], writes=[B_silc])

        S.stop("setup")

        def vcol(i, name, k=0):
            o = _voff(i, name) + k
            return vecs[:, o:o + 1]

        def xview(ap):
            return ap.rearrange("(k p) n -> p k n", p=128)

        def mod_alloc(ls):
            return ([sb(ls, f"wm{b}", [128, 8, 512], BF16) for b in range(2)], [S.buf(f"wm{b}", True) for b in range(2)])

        def mod_group(i, g, wm, B_wm):
            wv = W["w_mod"][i].rearrange("(k p) n -> p k n", p=128)
            b = g % 2
            S.dma("pool", wm[b][:], wv[:, :, g * 512:(g + 1) * 512], writes=[B_wm[b]])
            for jj in range(4):
                j = g * 4 + jj
                for k in range(8):
                    S.op("pe", lambda e: e.matmul(ps[5][:, j * 2:j * 2 + 2], wm[b][:, k, jj * 128:(jj + 1) * 128],
                                                  silc[:, k, :], start=(k == 0), stop=(k == 7)),
                         sig=(k == 7), reads=[B_wm[b], B_silc], writes=[B_ps[5]])

        def compute_mod(i):
            with ExitStack() as ls:
                wm, B_wm = mod_alloc(ls)
                for g in range(12):
                    mod_group(i, g, wm, B_wm)
                mod_finish(i)

        def mod_finish(i):
            modT, A_m, A_f, G_m, G_f, lamv, B_mod, B_lam = mods[i % 2]
            if True:
                psv = ps[5][:, 0:96].rearrange("p (j s) -> p j s", s=2)
                bm = vecs[:, _voff(i, "bmod"):_voff(i, "bmod") + 48]
                for s in range(2):
                    S.op("dve", lambda e: e.tensor_tensor(out=modT[:, :, s], in0=psv[:, :, s], in1=bm, op=ALU.add),
                         reads=[B_ps[5], B_vecs], writes=[B_mod])
                for s in range(2):
                    for (dst, sc_off, gname) in ((A_m, 8, "gpm"), (A_f, 32, "gpf")):
                        gv = vecs[:, _voff(i, gname):_voff(i, gname) + 8]
                        S.op("dve", lambda e: e.scalar_tensor_tensor(out=dst[:, :, s], in0=modT[:, sc_off:sc_off + 8, s], scalar=1.0,
                                                                     in1=gv, op0=ALU.add, op1=ALU.mult),
                             reads=[B_mod, B_vecs], writes=[B_mod])
                    for (dst, gt_off, gname) in ((G_m, 16, "gqm"), (G_f, 40, "gqf")):
                        gv = vecs[:, _voff(i, gname):_voff(i, gname) + 8]
                        S.op("dve", lambda e: e.tensor_tensor(out=dst[:, :, s], in0=modT[:, gt_off:gt_off + 8, s], in1=gv, op=ALU.mult),
                             reads=[B_mod, B_vecs], writes=[B_mod])
                lo = _voff(i, "lam")
                S.op("dve", lambda e: e.tensor_tensor(out=lamv[:, 2:3], in0=vecs[:, lo:lo + 1], in1=vecs[:, lo + 1:lo + 2], op=ALU.mult),
                     reads=[B_vecs], writes=[B_lam])
                S.op("dve", lambda e: e.tensor_tensor(out=lamv[:, 3:4], in0=vecs[:, lo + 2:lo + 3], in1=vecs[:, lo + 3:lo + 4], op=ALU.mult),
                     reads=[B_vecs, B_lam], writes=[B_lam])
                S.op("pe", lambda e: e.matmul(ps[6][:, 0:2], ones_f[:], lamv[:, 2:4], start=True, stop=True),
                     sig=True, reads=[B_lam, B_const], writes=[B_ps[6]])
                S.op("act", lambda e: e.activation(out=lamv[:, 2:4], in_=ps[6][:, 0:2], func=AF.Exp), reads=[B_ps[6]], writes=[B_lam])
                lam_init = 0.8 - 0.6 * math.exp(-0.3 * i)
                S.op("dve", lambda e: e.scalar_tensor_tensor(out=lamv[:, 0:1], in0=lamv[:, 3:4], scalar=-lam_init, in1=lamv[:, 2:3],
                                                             op0=ALU.add, op1=ALU.subtract), reads=[B_lam], writes=[B_lam])
                S.op("dve", lambda e: e.tensor_scalar(out=lamv[:, 1:2], in0=vcol(i, "gsub"), scalar1=(1.0 - lam_init), scalar2=None,
                                                      op0=ALU.mult), reads=[B_vecs, B_lam], writes=[B_lam])

        def rstd_from_ss(ss_ps_ap, B_ss, n_feat, dst, B_dst, tmp, B_tmp):
            S.op("act", lambda e: e.activation(out=tmp, in_=ss_ps_ap, func=AF.Sqrt, scale=1.0 / n_feat, bias=epsc[:, 0:1]),
                 reads=[B_ss, B_const], writes=[B_tmp])
            S.op("dve", lambda e: e.reciprocal(out=dst, in_=tmp), reads=[B_tmp], writes=[B_dst])

        epsc = sb(st, "epsc", [128, 1], F32)
        S.op("dve", lambda e: e.memset(epsc[:], EPS), writes=[B_const])

        def norm_load(ls_bufs, src_view, B_src, c0s, N):
            S.dma("sp", ls_bufs[0][:, :, 0:N], src_view[:, :, c0s:c0s + N], reads=[B_src], writes=[ls_bufs[1]])

        def norm_mod_chunk(ls_bufs, src_view, B_src, c0s, N, Acol, Bcol_off, s, out_bf, B_out, out_f32=None, do_load=True):
            xc, B_xc, sq, B_sq, rs, B_rs, tmp, B_tmp, t2, B_t2 = ls_bufs
            if do_load:
                S.dma("sp", xc[:, :, 0:N], src_view[:, :, c0s:c0s + N], reads=[B_src], writes=[B_xc])
            for k in range(8):
                S.op("act", lambda e: e.activation(out=sq[:, k, 0:N], in_=xc[:, k, 0:N], func=AF.Square), reads=[B_xc], writes=[B_sq])
            for k in range(8):
                S.op("pe", lambda e: e.matmul(ps[7][:, 0:N], ones_bf[:], sq[:, k, 0:N], start=(k == 0), stop=(k == 7)),
                     sig=(k == 7), reads=[B_sq, B_const], writes=[B_ps[7]])
            rstd_from_ss(ps[7][:, 0:N], B_ps[7], D, rs[:, 0:N], B_rs, tmp[:, 0:N], B_tmp)
            nt2 = t2.shape[1]
            for k in range(8):
                kk = k % nt2
                S.op("dve", lambda e: e.scalar_tensor_tensor(out=t2[:, kk, 0:N], in0=xc[:, k, 0:N], scalar=Acol[:, k, s:s + 1], in1=rs[:, 0:N],
                                                             op0=ALU.mult, op1=ALU.mult), reads=[B_xc, B_rs, B_mod], writes=[B_t2[kk]])
                dst = out_bf(k)
                S.op("act", lambda e: e.activation(out=dst, in_=t2[:, kk, 0:N], func=AF.Identity,
                                                   bias=modT[:, Bcol_off + k, s:s + 1], scale=1.0),
                     reads=[B_t2[kk], B_mod], writes=[B_out])

        def alloc_norm_bufs(ls, tag, nt2=2):
            xc = sb(ls, "xc" + tag, [128, 8, 512], F32)
            sq = sb(ls, "sq" + tag, [128, 8, 512], BF16)
            rs = sb(ls, "rs" + tag, [128, 512], F32)
            tmp = sb(ls, "tmp" + tag, [128, 512], F32)
            t2 = sb(ls, "t2" + tag, [128, nt2, 512], F32)
            return (xc, S.buf("xc" + tag, True), sq, S.buf("sq" + tag), rs, S.buf("rs" + tag), tmp, S.buf("tmp" + tag),
                    t2, [S.buf(f"t2{tag}{q}") for q in range(nt2)])

        x_src, B_xs = xT_in, B_xin
        y_src, B_ys = ctxT_in, B_cin
        for i in range(n_layers):
            last = i == DEPTH - 1
            lam_init = 0.8 - 0.6 * math.exp(-0.3 * i)
            x_mid, B_xm = xA, B_xA
            y_mid, B_ym = yA, B_yA
            x_dst, B_xd = (outT, B_out) if last else (xB, B_xB)
            y_dst, B_yd = yB, B_yB
            modT, A_m, A_f, G_m, G_f, lamv, B_mod, B_lam = mods[i % 2]
            if i == 0:
                compute_mod(0)
            S.stop(f"{i}:mod")

            with ExitStack() as lay:
                KT = sb(lay, "KT", [128, 4, NTOK], BF16)
                Vt = sb(lay, "Vt", [128, 34, 512], BF16)
                B_KT, B_Vt = S.buf("KT"), S.buf("Vt")
                with ExitStack() as pa:
                    HT = sb(pa, "HT", [128, 8, NTOK], BF16)
                    B_HT = S.buf("HT")
                    with ExitStack() as pa1:
                        nb0 = alloc_norm_bufs(pa1, "a0")
                        xc_b = sb(pa1, "xca1", [128, 8, 512], F32)
                        nb = [nb0, (xc_b, S.buf("xca1", True)) + tuple(nb0[2:])]

                        def a1_load(cj):
                            c0j, Nj, iscj = CHUNKS[cj]
                            norm_load(nb[cj % 2], xview(y_src if iscj else x_src), B_ys if iscj else B_xs, 0 if iscj else c0j, Nj)
                        a1_load(0)
                        for ci, (c0, N, isc) in enumerate(CHUNKS):
                            if ci + 1 < len(CHUNKS):
                                a1_load(ci + 1)
                            src = xview(y_src if isc else x_src)
                            norm_mod_chunk(nb[ci % 2], src, B_ys if isc else B_xs, 0 if isc else c0, N, A_m, 0, 1 if isc else 0,
                                           lambda k: HT[:, k, c0:c0 + N], B_HT, do_load=False)
                    S.barrier()
                    S.stop(f"{i}:A1")
                    if dbg and i == 0:
                        dbg_HT = nc.dram_tensor("dbg_HT", [8, 128, NTOK], BF16, kind="ExternalOutput").ap()
                        B_dbg = S.buf("dbg", True)
                        for k in range(8):
                            S.dma("sp", dbg_HT[k], HT[:, k, :], reads=[B_HT], writes=[B_dbg])
                    with ExitStack() as pa2:
                        wt = [sb(pa2, f"wt{b}", [128, 8, 512], BF16) for b in range(2)]
                        B_wt = [S.buf(f"wt{b}", True) for b in range(2)]
                        NST = 12
                        stg = [sb(pa2, f"stg{b}", [128, 512], BF16) for b in range(NST)]
                        B_stg = [S.buf(f"stg{b}", True) for b in range(NST)]
                        qb = [sb(pa2, f"qb{b}", [128, 512], BF16) for b in range(2)]
                        B_qb = [S.buf(f"qb{b}") for b in range(2)]
                        r1 = [sb(pa2, f"r1{b}", [128, 512], F32) for b in range(2)]
                        B_r1 = [S.buf(f"r1{b}") for b in range(2)]
                        r2 = [sb(pa2, f"r2{b}", [128, 512], F32) for b in range(2)]
                        B_r2 = [S.buf(f"r2{b}") for b in range(2)]
                        wv = W["w_in"][i].rearrange("(k p) n -> p k n", p=128)
                        cosb = sb(pa2, "cosb", [128, T], BF16)
                        sinb = sb(pa2, "sinb", [128, T], BF16)
                        B_tab = S.buf("ropetab", True)
                        for hh in range(4):
                            S.dma("pool", cosb[:, hh * 1024:(hh + 1) * 1024], cos_in[:, hh * 1024:(hh + 1) * 1024], writes=[B_tab])
                            S.dma("pool", sinb[:, hh * 1024:(hh + 1) * 1024], sin_in[:, hh * 1024:(hh + 1) * 1024], writes=[B_tab])
                        cnt = 0
                        import os
                        glist = [int(v) for v in os.environ.get("KDBG_G", "0,1,2,3,4,5,6,7,8,9,10,11,12").split(",")]
                        for g in glist:
                            b = g % 2
                            S.dma("pool", wt[b][:], wv[:, :, g * 512:(g + 1) * 512], writes=[B_wt[b]])
                            if g == 2:
                                for tt in range(34):
                                    pb_ = 4 + (tt % 2)
                                    for k in range(8):
                                        S.op("pe", lambda e: e.matmul(ps[pb_][:], HT[:, k, tt * 128:(tt + 1) * 128], wt[b][:, k, :],
                                                                      start=(k == 0), stop=(k == 7)),
                                             sig=(k == 7), reads=[B_HT, B_wt[b]], writes=[B_ps[pb_]])
                                    if tt % 2 == 0:
                                        S.op("act", lambda e: e.activation(out=Vt[:, tt, :], in_=ps[pb_][:], func=AF.Copy),
                                             reads=[B_ps[pb_]], writes=[B_Vt])
                                    else:
                                        S.op("dve", lambda e: e.tensor_copy(out=Vt[:, tt, :], in_=ps[pb_][:]),
                                             reads=[B_ps[pb_]], writes=[B_Vt])
                                continue
                            for jj in range(4):
                                j = g * 4 + jj
                                for ci, (c0, N, isc) in enumerate(CHUNKS):
                                    if isc and last and g != 1:
                                        continue
                                    pb_ = cnt % 4
                                    cnt += 1
                                    for k in range(8):
                                        S.op("pe", lambda e: e.matmul(ps[pb_][:, 0:N], wt[b][:, k, jj * 128:(jj + 1) * 128], HT[:, k, c0:c0 + N],
                                                                      start=(k == 0), stop=(k == 7)),
                                             sig=(k == 7), reads=[B_HT, B_wt[b]], writes=[B_ps[pb_]])
                                    if g <= 1:
                                        if g == 0:
                                            sidx = cnt % NST
                                            dst, B_dst = stg[sidx][:, 0:N], B_stg[sidx]
                                        else:
                                            dst, B_dst = KT[:, jj, c0:c0 + N], B_KT
                                        kv = os.environ.get("KDBG_V", "4")
                                        if isc or kv == "1":
                                            S.op("act", lambda e: e.activation(out=dst, in_=ps[pb_][:, 0:N], func=AF.Copy),
                                                 reads=[B_ps[pb_]], writes=[B_dst])
                                        else:
                                            rb = cnt % 2
                                            S.op("act", lambda e: e.activation(out=qb[rb][:, 0:N], in_=ps[pb_][:, 0:N], func=AF.Copy),
                                                 reads=[B_ps[pb_]], writes=[B_qb[rb]])
                                            pp_ = 4 + rb
                                            if kv != "2":
                                                S.op("pe", lambda e: e.matmul(ps[pp_][:, 0:N], pm[:], qb[rb][:, 0:N], start=True, stop=True),
                                                     sig=True, reads=[B_qb[rb], B_const], writes=[B_ps[pp_]])
                                            else:
                                                pp_ = pb_
                                            if kv not in ("3", "4"):
                                                S.op("dve", lambda e: e.tensor_tensor(out=r1[rb][:, 0:N], in0=ps[pb_][:, 0:N], in1=cosb[:, c0:c0 + N], op=ALU.mult),
                                                     reads=[B_ps[pb_], B_tab], writes=[B_r1[rb]])
                                                S.op("dve", lambda e: e.tensor_tensor(out=r2[rb][:, 0:N], in0=ps[pp_][:, 0:N], in1=sinb[:, c0:c0 + N], op=ALU.mult),
                                                     reads=[B_ps[pp_], B_tab], writes=[B_r2[rb]])
                                            else:
                                                S.op("act", lambda e: e.activation(out=r1[rb][:, 0:N], in_=ps[pb_][:, 0:N], func=AF.Copy),
                                                     reads=[B_ps[pb_], B_tab], writes=[B_r1[rb]])
                                                S.op("act", lambda e: e.activation(out=r2[rb][:, 0:N], in_=ps[pp_][:, 0:N], func=AF.Copy),
                                                     reads=[B_ps[pp_], B_tab], writes=[B_r2[rb]])
                                            if kv == "4":
                                                S.op("dve", lambda e: e.tensor_tensor(out=r1[rb][:, 0:N], in0=r1[rb][:, 0:N], in1=cosb[:, c0:c0 + N], op=ALU.mult),
                                                     reads=[B_r1[rb], B_tab], writes=[B_r1[rb]])
                                                S.op("dve", lambda e: e.tensor_tensor(out=r2[rb][:, 0:N], in0=r2[rb][:, 0:N], in1=sinb[:, c0:c0 + N], op=ALU.mult),
                                                     reads=[B_r2[rb], B_tab], writes=[B_r2[rb]])
                                            S.op(os.environ.get("KDBG_ROPE", "pool"), lambda e: e.tensor_tensor(out=dst, in0=r1[rb][:, 0:N], in1=r2[rb][:, 0:N], op=ALU.add),
                                                 reads=[B_r1[rb], B_r2[rb]], writes=[B_dst])
                                        if g == 0:
                                            S.dma("sp", QT[jj, :, c0:c0 + N], dst, reads=[B_dst], writes=[B_QT], sem_buf=B_dst)
                                    else:
                                        sidx = cnt % NST
                                        zc0 = (ZC if isc else ZL + c0)
                                        if g >= 7:
                                            S.op("act", lambda e: e.activation(out=stg[sidx][:, 0:N], in_=ps[pb_][:, 0:N], func=AF.Sigmoid),
                                                 reads=[B_ps[pb_]], writes=[B_stg[sidx]])
                                        elif cnt % 2 == 0:
                                            S.op("act", lambda e: e.activation(out=stg[sidx][:, 0:N], in_=ps[pb_][:, 0:N], func=AF.Copy),
                                                 reads=[B_ps[pb_]], writes=[B_stg[sidx]])
                                        else:
                                            S.op("dve", lambda e: e.tensor_copy(out=stg[sidx][:, 0:N], in_=ps[pb_][:, 0:N]),
                                                 reads=[B_ps[pb_]], writes=[B_stg[sidx]])
                                        S.dma("sp", ZT[j - 12, :, zc0:zc0 + N], stg[sidx][:, 0:N], reads=[B_stg[sidx]], writes=[B_ZT], sem_buf=B_stg[sidx])
                    S.barrier()
                    S.stop(f"{i}:A2")
                with ExitStack() as pb:
                    qz = [[sb(pb, f"qz{m}{b}", [128, 4, 512], BF16) for b in range(2)] for m in range(2)]
                    B_qz = [S.buf(f"qz{b}", True) for b in range(2)]
                    for b_ in range(2):
                        S.op("pool", lambda e: e.memset(qz[0][b_][64:128, :, :], 0.0), writes=[B_qz[b_]])
                        S.op("pool", lambda e: e.memset(qz[1][b_][0:64, :, :], 0.0), writes=[B_qz[b_]])
                    Pt = [[sb(pb, f"P{m}{b}", [128, 512], BF16) for b in range(3)] for m in range(2)]
                    B_P = [[S.buf(f"P{m}{b}") for b in range(3)] for m in range(2)]
                    pacc = [sb(pb, f"pacc{m}", [128, 512], F32) for m in range(2)]
                    B_pacc = [S.buf(f"pacc{m}") for m in range(2)]
                    first_ci = 0

                    def load_q(cj):
                        c0j, Nj, _ = CHUNKS[cj]
                        bj = cj % 2
                        S.dma("sp", qz[0][bj][0:64, :, 0:Nj], QT[:, 0:64, c0j:c0j + Nj].rearrange("h p n -> p h n"), reads=[B_QT], writes=[B_qz[bj]])
                        S.dma("sp", qz[1][bj][64:128, :, 0:Nj], QT[:, 64:128, c0j:c0j + Nj].rearrange("h p n -> p h n"), reads=[B_QT], writes=[B_qz[bj]])
                    rl = [sb(pb, f"rl{m}", [128, 512], F32) for m in range(2)]
                    B_rl = [S.buf(f"rl{m}") for m in range(2)]
                    a1 = sb(pb, "a1", [128, 512], F32); B_a1 = S.buf("a1")
                    a2 = sb(pb, "a2", [128, 512], F32); B_a2 = S.buf("a2")
                    aa = sb(pb, "aa", [128, 512], F32); B_aa = S.buf("aa")
                    asq = sb(pb, "asq", [128, 512], BF16); B_asq = S.buf("asq")
                    tmpb = sb(pb, "tmpb", [128, 512], F32); B_tmpb = S.buf("tmpb")
                    rsb = sb(pb, "rsb", [128, 512], F32); B_rsb = S.buf("rsb")
                    attn_o = sb(pb, "attn_o", [128, 4, 512], BF16); B_ao = S.buf("attn_o")
                    for ci, (c0, N, isc) in enumerate(CHUNKS):
                        if isc and last:
                            continue
                        s = 1 if isc else 0
                        qb_ = ci % 2
                        if ci == first_ci:
                            load_q(ci)
                        nxt = [c_ for c_ in range(ci + 1, len(CHUNKS)) if not (CHUNKS[c_][2] and last)]
                        if nxt:
                            load_q(nxt[0])
                        kts = [32, 33] if isc else list(range(34))
                        nk = len(kts)
                        SB = [[0, 1], [2, 3]]
                        NSB = 2
                        LB = [6, 7]

                        def qk(h, idx):
                            kt = kts[idx]
                            for m in range(2):
                                bank = SB[m][idx % NSB]
                                S.op("pe", lambda e: e.matmul(ps[bank][:, 0:N], KT[:, h, kt * 128:(kt + 1) * 128], qz[m][qb_][:, h, 0:N], start=True, stop=True),
                                     sig=True, reads=[B_KT, B_qz[qb_]], writes=[B_ps[bank]])

                        def post(h):
                            for m in range(2):
                                S.op("dve", lambda e: e.reciprocal(out=rl[m][:, 0:N], in_=ps[LB[m]][:, 0:N]), reads=[B_ps[LB[m]]], writes=[B_rl[m]])
                            S.op("dve", lambda e: e.tensor_tensor(out=a1[:, 0:N], in0=ps[4][:, 0:N], in1=rl[0][:, 0:N], op=ALU.mult),
                                 reads=[B_ps[4], B_rl[0]], writes=[B_a1])
                            S.op("dve", lambda e: e.tensor_tensor(out=a2[:, 0:N], in0=ps[5][:, 0:N], in1=rl[1][:, 0:N], op=ALU.mult),
                                 reads=[B_ps[5], B_rl[1]], writes=[B_a2])
                            S.op("dve", lambda e: e.scalar_tensor_tensor(out=aa[:, 0:N], in0=a2[:, 0:N], scalar=lamv[:, 0:1], in1=a1[:, 0:N],
                                                                         op0=ALU.mult, op1=ALU.add), reads=[B_a1, B_a2, B_lam], writes=[B_aa])
                            S.op("act", lambda e: e.activation(out=asq[:, 0:N], in_=aa[:, 0:N], func=AF.Square), reads=[B_aa], writes=[B_asq])
                            S.op("pe", lambda e: e.matmul(ps[LB[1]][:, 0:N], ones_bf[:], asq[:, 0:N], start=True, stop=True),
                                 sig=True, reads=[B_asq, B_const], writes=[B_ps[LB[1]]])
                            rstd_from_ss(ps[LB[1]][:, 0:N], B_ps[LB[1]], 128, rsb[:, 0:N], B_rsb, tmpb[:, 0:N], B_tmpb)
                            S.op("dve", lambda e: e.scalar_tensor_tensor(out=attn_o[:, h, 0:N], in0=aa[:, 0:N], scalar=lamv[:, 1:2], in1=rsb[:, 0:N],
                                                                         op0=ALU.mult, op1=ALU.mult), reads=[B_aa, B_rsb, B_lam], writes=[B_ao])

                        qk(0, 0)
                        if nk > 1:
                            qk(0, 1)
                        for h in range(4):
                            for idx in range(nk):
                                kt = kts[idx]
                                if idx + 1 < nk and idx >= 1:
                                    qk(h, idx + 1)
                                for m in range(2):
                                    bank = SB[m][idx % NSB]
                                    S.op("act", lambda e: e.activation(out=Pt[m][idx % 3][:, 0:N], in_=ps[bank][:, 0:N], func=AF.Exp, scale=0.125),
                                         reads=[B_ps[bank]], writes=[B_P[m][idx % 3]])
                                for m in range(2):
                                    S.op("pe", lambda e: e.matmul(ps[4 + m][:, 0:N], Vt[:, kt, h * 128:(h + 1) * 128], Pt[m][idx % 3][:, 0:N],
                                                                  start=(idx == 0), stop=(idx == nk - 1)),
                                         sig=(idx == nk - 1), reads=[B_Vt, B_P[m][idx % 3]], writes=[B_ps[4 + m]])
                                    if m == 0:
                                        S.op("pe", lambda e: e.matmul(ps[LB[0]][:, 0:N], ones_bf[:], Pt[0][idx % 3][:, 0:N],
                                                                      start=(idx == 0), stop=(idx == nk - 1)),
                                             sig=(idx == nk - 1), reads=[B_const, B_P[0][idx % 3]], writes=[B_ps[LB[0]]])
                                    else:
                                        q_ = idx % 2
                                        en_ = "dve" if q_ == 0 else "pool"
                                        if idx < 2:
                                            S.op(en_, lambda e: e.tensor_copy(out=pacc[q_][:, 0:N], in_=Pt[1][idx % 3][:, 0:N]), reads=[B_P[1][idx % 3]], writes=[B_pacc[q_]])
                                        else:
                                            S.op(en_, lambda e: e.tensor_tensor(out=pacc[q_][:, 0:N], in0=pacc[q_][:, 0:N], in1=Pt[1][idx % 3][:, 0:N], op=ALU.add),
                                                 reads=[B_P[1][idx % 3], B_pacc[q_]], writes=[B_pacc[q_]])
                            S.op("pe", lambda e: e.matmul(ps[LB[1]][:, 0:N], ones_f[:], pacc[0][:, 0:N], start=True, stop=False),
                                 sig=False, reads=[B_pacc[0], B_const], writes=[B_ps[LB[1]]])
                            S.op("pe", lambda e: e.matmul(ps[LB[1]][:, 0:N], ones_f[:], pacc[1][:, 0:N], start=False, stop=True),
                                 sig=True, reads=[B_pacc[1], B_const], writes=[B_ps[LB[1]]])
                            if h + 1 < 4:
                                qk(h + 1, 0)
                                if nk > 1:
                                    qk(h + 1, 1)
                            post(h)
                        S.dma("sp", AOT[:, :, c0:c0 + N].rearrange("h p n -> p h n"), attn_o[:, :, 0:N], reads=[B_ao], writes=[B_AOT])
                    S.barrier()
                    S.stop(f"{i}:B1")
            with ExitStack() as pb:
                wbr = sb(pb, "wbr", [128, 12, 1024], BF16)
                wout = sb(pb, "wout", [128, 8, 1024], BF16)
                poolw = sb(pb, "poolw", [128, 4, 128], BF16)
                B_wB = S.buf("wB", True)
                for r in range(3):
                    S.dma("pool", wbr[:, r * 4:(r + 1) * 4, :], W["w_branch"][i, r].rearrange("(k p) n -> p k n", p=128), writes=[B_wB])
                S.dma("pool", wout[:], W["w_out"][i].rearrange("(k p) n -> p k n", p=128), writes=[B_wB])
                S.dma("pool", poolw[:], W["pool_w"][i].rearrange("g c d -> c g d"), writes=[B_wB])
                attn_o = sb(pb, "attn_o", [128, 4, 512], BF16); B_ao = S.buf("attn_o2", True)
                tmpb = sb(pb, "tmpb", [128, 512], F32); B_tmpb = S.buf("tmpb")
                rsb = sb(pb, "rsb", [128, 512], F32); B_rsb = S.buf("rsb")
                conv_o = sb(pb, "conv_o", [128, 4, 512], BF16); B_co = S.buf("conv_o")
                pool_o = sb(pb, "pool_o", [128, 4, 512], BF16); B_po = S.buf("pool_o")
                cbt = sb(pb, "cbt", [128, 4, 512], BF16); B_cbt = S.buf("cbt", True)
                cct = sb(pb, "cct", [128, 4, 514], BF16); B_cct = S.buf("cct", True)
                cxt = sb(pb, "cxt", [128, 4, 514], BF16); B_cxt = S.buf("cxt", True)
                ut = sb(pb, "ut", [128, 4, 514], F32); B_ut = S.buf("ut")
                cv = sb(pb, "cv", [128, 4, 512], F32); B_cv = S.buf("cv")
                pint = sb(pb, "pint", [128, 4, 528], BF16); B_pin = S.buf("pint", True)
                w1 = sb(pb, "w1", [128, 4, 528], F32); B_w1 = [S.buf(f"w1{g}") for g in range(4)]
                w2 = sb(pb, "w2", [128, 4, 528], F32); B_w2 = [S.buf(f"w2{g}") for g in range(4)]
                ppb = sb(pb, "ppb", [128, 4, 512], BF16); B_pp = S.buf("ppb")
                invs = sb(pb, "invs", [128, 4, 512], F32); B_inv = S.buf("invs", True)
                mg2 = [[sb(pb, f"mg{r}{b}", [128, 512], F32) for r in range(3)] for b in range(2)]
                B_mg2 = [[S.buf(f"mg{r}{b}") for r in range(3)] for b in range(2)]
                B_mixj = [S.buf(f"mixT{j}") for j in range(8)]
                merged = sb(pb, "merged", [128, 8, 512], BF16); B_mer = S.buf("merged")
                mixT = sb(pb, "mixT", [128, 8, 512], F32); B_mix = S.buf("mixT")
                msq = sb(pb, "msq", [128, 8, 512], BF16); B_msq = S.buf("msq")
                xcb = sb(pb, "xcb", [128, 8, 512], F32); B_xcb = S.buf("xcb", True)

                conv_o2 = [conv_o, sb(pb, "conv_o_b", [128, 4, 512], BF16)]; B_co2 = [B_co, S.buf("conv_o_b")]
                ppb2 = [ppb, sb(pb, "ppb_b", [128, 4, 512], BF16)]; B_pp2 = [B_pp, S.buf("ppb_b")]
                gj = [sb(pb, f"gj{b}", [128, 3, 512], BF16) for b in range(2)]; B_gj = [S.buf(f"gj{b}", True) for b in range(2)]
                cw = _voff(i, "convw")
                pso = _voff(i, "pscale")
                b2_chunks = [ci for ci, c in enumerate(CHUNKS) if not (c[2] and last)]

                def load_ao(ci):
                    c0, N, isc = CHUNKS[ci]
                    S.dma("sp", attn_o[:, :, 0:N], AOT[:, :, c0:c0 + N].rearrange("h p n -> p h n"), reads=[B_AOT], writes=[B_ao])

                def stageX(ci):
                    c0, N, isc = CHUNKS[ci]
                    xb = ci % 2
                    zc0 = ZC if isc else ZL + c0
                    S.dma("sp", cbt[:, :, 0:N], ZT[0:4, :, zc0:zc0 + N].rearrange("j p n -> p j n"), reads=[B_ZT], writes=[B_cbt])
                    S.dma("sp", cct[:, :, 0:N + 2], ZT[4:8, :, zc0 - 1:zc0 + N + 1].rearrange("j p n -> p j n"), reads=[B_ZT], writes=[B_cct])
                    S.dma("sp", cxt[:, :, 0:N + 2], ZT[8:12, :, zc0 - 1:zc0 + N + 1].rearrange("j p n -> p j n"), reads=[B_ZT], writes=[B_cxt])
                    S.dma("sp", pint[:, :, 0:N + 16], ZT[12:16, :, zc0 - 8:zc0 + N + 8].rearrange("j p n -> p j n"), reads=[B_ZT], writes=[B_pin])
                    tbl = 2 if isc else (0 if ci == 0 else (1 if ci == 7 else None))
                    if tbl is not None:
                        S.dma("sp", invs[:], invt_in[:, tbl], writes=[B_inv])
                    S.op("pool", lambda e: e.tensor_tensor(out=ut[:, :, 0:N + 2], in0=cct[:, :, 0:N + 2], in1=cxt[:, :, 0:N + 2], op=ALU.mult),
                         reads=[B_cct, B_cxt], writes=[B_ut])

                    def padd(dst, B_d, a, b_, Bs):
                        S.op("pool", lambda e: e.tensor_tensor(out=dst, in0=a, in1=b_, op=ALU.add), reads=Bs, writes=[B_d])
                    M = N
                    padd(w1[:, 0, 8:M + 8], B_w1[0], pint[:, 0, 7:M + 7], pint[:, 0, 8:M + 8], [B_pin])
                    for g in range(1, 4):
                        padd(w1[:, g, 1:M + 16], B_w1[g], pint[:, g, 0:M + 15], pint[:, g, 1:M + 16], [B_pin])
                    padd(w2[:, 1, 8:M + 8], B_w2[1], w1[:, 1, 7:M + 7], w1[:, 1, 9:M + 9], [B_w1[1]])
                    for g in (2, 3):
                        padd(w2[:, g, 2:M + 15], B_w2[g], w1[:, g, 1:M + 14], w1[:, g, 3:M + 16], [B_w1[g]])
                    padd(w1[:, 2, 8:M + 8], B_w1[2], w2[:, 2, 6:M + 6], w2[:, 2, 10:M + 10], [B_w2[2]])
                    padd(w1[:, 3, 4:M + 13], B_w1[3], w2[:, 3, 2:M + 11], w2[:, 3, 6:M + 15], [B_w2[3]])
                    padd(w2[:, 3, 8:M + 8], B_w2[3], w1[:, 3, 4:M + 4], w1[:, 3, 12:M + 12], [B_w1[3]])
                    for ct in range(4):
                        S.op("dve", lambda e: e.tensor_scalar(out=cv[:, ct, 0:N], in0=ut[:, ct, 1:N + 1], scalar1=vecs[:, cw + 4 + ct:cw + 5 + ct],
                                                              scalar2=None, op0=ALU.mult), reads=[B_ut, B_vecs], writes=[B_cv])
                        S.op("dve", lambda e: e.scalar_tensor_tensor(out=cv[:, ct, 0:N], in0=ut[:, ct, 0:N], scalar=vecs[:, cw + ct:cw + ct + 1],
                                                                     in1=cv[:, ct, 0:N], op0=ALU.mult, op1=ALU.add),
                             reads=[B_ut, B_vecs, B_cv], writes=[B_cv])
                        S.op("dve", lambda e: e.scalar_tensor_tensor(out=cv[:, ct, 0:N], in0=ut[:, ct, 2:N + 2], scalar=vecs[:, cw + 8 + ct:cw + 9 + ct],
                                                                     in1=cv[:, ct, 0:N], op0=ALU.mult, op1=ALU.add),
                             reads=[B_ut, B_vecs, B_cv], writes=[B_cv])
                    S.op("pool", lambda e: e.tensor_tensor(out=conv_o2[xb][:, :, 0:N], in0=cv[:, :, 0:N], in1=cbt[:, :, 0:N], op=ALU.mult),
                         reads=[B_cv, B_cbt], writes=[B_co2[xb]])
                    fin = [(w1, B_w1[0]), (w2, B_w2[1]), (w1, B_w1[2]), (w2, B_w2[3])]
                    for g, wsz in enumerate((2, 4, 8, 16)):
                        src_t, B_s = fin[g]
                        if tbl is None:
                            S.op("dve", lambda e: e.scalar_tensor_tensor(out=ppb2[xb][:, g, 0:N], in0=src_t[:, g, 8:N + 8], scalar=1.0 / wsz,
                                                                         in1=pint[:, g, 8:N + 8], op0=ALU.mult, op1=ALU.subtract),
                                 reads=[B_s, B_pin], writes=[B_pp2[xb]])
                        else:
                            S.op("dve", lambda e: e.tensor_tensor(out=src_t[:, g, 8:N + 8], in0=src_t[:, g, 8:N + 8], in1=invs[:, g, 0:N], op=ALU.mult),
                                 reads=[B_s, B_inv], writes=[B_s])
                            S.op("dve", lambda e: e.tensor_tensor(out=ppb2[xb][:, g, 0:N], in0=src_t[:, g, 8:N + 8], in1=pint[:, g, 8:N + 8], op=ALU.subtract),
                                 reads=[B_s, B_pin], writes=[B_pp2[xb]])

                def load_g(ci, j):
                    c0, N, isc = CHUNKS[ci]
                    zc0 = ZC if isc else ZL + c0
                    for r in range(3):
                        S.dma("sp", gj[j % 2][:, r, 0:N], ZT[16 + r * 8 + j, :, zc0:zc0 + N], reads=[B_ZT], writes=[B_gj[j % 2]])

                def stageY(ci, nxt):
                    c0, N, isc = CHUNKS[ci]
                    s = 1 if isc else 0
                    xb = ci % 2
                    srcv = xview(y_src if isc else x_src)
                    cs = 0 if isc else c0
                    S.dma("sp", xcb[:, :, 0:N], srcv[:, :, cs:cs + N], reads=[B_ys if isc else B_xs], writes=[B_xcb])
                    load_g(ci, 0)
                    for g in range(4):
                        S.op("pe", lambda e: e.matmul(ps[0][:, 0:N], poolw[:, g, :], ppb2[xb][:, g, 0:N], start=True, stop=True), sig=True,
                             reads=[B_wB, B_pp2[xb]], writes=[B_ps[0]])
                        S.op("act", lambda e: e.activation(out=pool_o[:, g, 0:N], in_=ps[0][:, 0:N], func=AF.Identity, scale=vecs[:, pso + g:pso + g + 1]),
                             reads=[B_ps[0], B_vecs], writes=[B_po])
                    brs = [(attn_o, B_ao), (conv_o2[xb], B_co2[xb]), (pool_o, B_po)]
                    bset = [[1, 2, 3], [4, 5, 6]]

                    def branches(j):
                        for r in range(3):
                            bk = bset[j % 2][r]
                            for k in range(4):
                                S.op("pe", lambda e: e.matmul(ps[bk][:, 0:N], wbr[:, r * 4 + k, j * 128:(j + 1) * 128], brs[r][0][:, k, 0:N],
                                                              start=(k == 0), stop=(k == 3)), sig=(k == 3),
                                     reads=[B_wB, brs[r][1]], writes=[B_ps[bk]])
                    branches(0)
                    for j in range(8):
                        if j + 1 < 8:
                            load_g(ci, j + 1)
                        mg, B_mg = mg2[j % 2], B_mg2[j % 2]
                        mgb, B_mgb = mg, B_mg
                        for r in range(3):
                            bk = bset[j % 2][r]
                            S.op("act", lambda e: e.activation(out=mg[r][:, 0:N], in_=ps[bk][:, 0:N], func=AF.Copy),
                                 reads=[B_ps[bk]], writes=[B_mg[r]])
                            S.op("dve", lambda e: e.tensor_tensor(out=mgb[r][:, 0:N], in0=mg[r][:, 0:N], in1=gj[j % 2][:, r, 0:N], op=ALU.mult),
                                 reads=[B_mg[r], B_gj[j % 2]], writes=[B_mgb[r]])
                        if j + 1 < 8:
                            branches(j + 1)
                        S.op("pool", lambda e: e.tensor_tensor(out=mgb[0][:, 0:N], in0=mgb[0][:, 0:N], in1=mgb[1][:, 0:N], op=ALU.add),
                             reads=[B_mgb[0], B_mgb[1]], writes=[B_mgb[0]])
                        S.op("pool", lambda e: e.tensor_tensor(out=merged[:, j, 0:N], in0=mgb[0][:, 0:N], in1=mgb[2][:, 0:N], op=ALU.add),
                             reads=[B_mgb[0], B_mgb[2]], writes=[B_mer])
                    if nxt is not None:
                        load_ao(nxt)
                    for j in range(8):
                        bank = 4 + (j % 2)
                        for k in range(8):
                            S.op("pe", lambda e: e.matmul(ps[bank][:, 0:N], wout[:, k, j * 128:(j + 1) * 128], merged[:, k, 0:N],
                                                          start=(k == 0), stop=(k == 7)), sig=(k == 7),
                                 reads=[B_wB, B_mer], writes=[B_ps[bank]])
                        S.op("act", lambda e: e.activation(out=mixT[:, j, 0:N], in_=ps[bank][:, 0:N], func=AF.Copy), reads=[B_ps[bank]], writes=[B_mixj[j]])
                        S.op("act", lambda e: e.activation(out=msq[:, j, 0:N], in_=ps[bank][:, 0:N], func=AF.Square), reads=[B_ps[bank]], writes=[B_msq])
                    for j in range(8):
                        S.op("pe", lambda e: e.matmul(ps[6][:, 0:N], ones_bf[:], msq[:, j, 0:N], start=(j == 0), stop=(j == 7)), sig=(j == 7),
                             reads=[B_msq, B_const], writes=[B_ps[6]])
                    rstd_from_ss(ps[6][:, 0:N], B_ps[6], D, rsb[:, 0:N], B_rsb, tmpb[:, 0:N], B_tmpb)
                    for j in range(8):
                        S.op("dve", lambda e: e.scalar_tensor_tensor(out=mixT[:, j, 0:N], in0=mixT[:, j, 0:N], scalar=G_m[:, j, s:s + 1], in1=rsb[:, 0:N],
                                                                     op0=ALU.mult, op1=ALU.mult), reads=[B_mixj[j], B_rsb, B_mod], writes=[B_mixj[j]])
                        S.op("pool", lambda e: e.tensor_tensor(out=mixT[:, j, 0:N], in0=mixT[:, j, 0:N], in1=xcb[:, j, 0:N], op=ALU.add),
                             reads=[B_mixj[j], B_xcb], writes=[B_mixj[j]])
                    dstv = xview(y_mid if isc else x_mid)
                    S.dma("sp", dstv[:, :, cs:cs + N], mixT[:, :, 0:N], reads=B_mixj, writes=[B_ym if isc else B_xm])

                load_ao(b2_chunks[0])
                stageX(b2_chunks[0])
                for n_, ci in enumerate(b2_chunks):
                    nxt = b2_chunks[n_ + 1] if n_ + 1 < len(b2_chunks) else None
                    if nxt is not None:
                        stageX(nxt)
                    stageY(ci, nxt)
                S.barrier()
                S.stop(f"{i}:B2")
            n_exp = 1 if i % 2 == 0 else NE
            moe = n_exp > 1
            ffn_chunks = [c for c in CHUNKS if not (c[2] and last)]
            with ExitStack() as pc1:
                nbf = alloc_norm_bufs(pc1, "c", 8)
                h2s = sb(pc1, "h2s", [128, 8, 512], BF16); B_h2s = S.buf("h2s")
                if moe:
                    ro = 16 + DEPTH * VEC_LAYER
                    lg = sb(pc1, "lg", [128, 8], F32); B_lg = S.buf("lg")
                    l2 = sb(pc1, "l2", [128, 8], F32); B_l2 = S.buf("l2")
                    mk1 = sb(pc1, "mk1", [128, 8], F32); B_mk1 = S.buf("mk1")
                    mk2 = sb(pc1, "mk2", [128, 8], F32); B_mk2 = S.buf("mk2")
                    st1 = sb(pc1, "st1", [128, 8], F32); B_st = S.buf("st1")
                    comb = sb(pc1, "comb", [128, 8], F32); B_comb = S.buf("comb")
                    combT = sb(pc1, "combT", [8, 512], F32); B_combT = S.buf("combT")
                    h2f = sb(pc1, "h2f", [128, 8, 512], F32); B_h2f = S.buf("h2f")
                    if SPARSE_MOE:
                        sel = sb(pc1, "sel", [128, 8], F32); B_sel = S.buf("sel")
                        selsum = sb(pc1, "selsum", [128, 8], F32); B_selsum = S.buf("selsum")
                        tmp8 = sb(pc1, "tmp8", [128, 2, 8], F32); B_tmp8 = S.buf("tmp8")
                        h2rows = sb(pc1, "h2rows", [128, 32, 1024], BF16); B_h2rows = S.buf("h2rows")
                        rt = sb(pc1, "rt", [128, 8, 32], F32); B_rt = S.buf("rt")
                        first_tile = [True]
                for ci, (c0, N, isc) in enumerate(ffn_chunks):
                    src = xview(y_mid if isc else x_mid)
                    norm_mod_chunk(nbf, src, B_ym if isc else B_xm, 0 if isc else c0, N, A_f, 24, 1 if isc else 0,
                                   lambda k: h2s[:, k, 0:N], B_h2s)
                    S.dma("sp", H2T[:, :, c0:c0 + N].rearrange("k p n -> p k n"), h2s[:, :, 0:N], reads=[B_h2s], writes=[B_H2T])
                    if moe:
                        t2 = nbf[8]; B_t2 = nbf[9]
                        for k in range(8):
                            S.op("dve", lambda e: e.tensor_scalar(out=h2f[:, k, 0:N], in0=t2[:, k, 0:N], scalar1=modT[:, 24 + k, 0:1], scalar2=None, op0=ALU.add),
                                 reads=[B_t2[k], B_mod], writes=[B_h2f])
                        for tt in range(N // 128):
                            for k in range(8):
                                S.op("pe", lambda e: e.matmul(ps[0][:, 0:8], h2f[:, k, tt * 128:(tt + 1) * 128], vecs[:, ro + k * 8:ro + k * 8 + 8],
                                                              start=(k == 0), stop=(k == 7)), sig=(k == 7), reads=[B_h2f, B_vecs], writes=[B_ps[0]])
                            S.op("dve", lambda e: e.tensor_copy(out=lg[:], in_=ps[0][:, 0:8]), reads=[B_ps[0]], writes=[B_lg])
                            S.op("dve", lambda e: e.reduce_max(out=st1[:, 0:1], in_=lg[:], axis=mybir.AxisListType.X), reads=[B_lg], writes=[B_st])
                            S.op("dve", lambda e: e.tensor_scalar(out=mk1[:], in0=lg[:], scalar1=st1[:, 0:1], scalar2=None, op0=ALU.is_equal),
                                 reads=[B_lg, B_st], writes=[B_mk1])
                            S.op("dve", lambda e: e.scalar_tensor_tensor(out=l2[:], in0=mk1[:], scalar=-1e30, in1=lg[:], op0=ALU.mult, op1=ALU.add),
                                 reads=[B_mk1, B_lg], writes=[B_l2])
                            S.op("dve", lambda e: e.reduce_max(out=st1[:, 1:2], in_=l2[:], axis=mybir.AxisListType.X), reads=[B_l2, B_st], writes=[B_st])
                            S.op("dve", lambda e: e.tensor_scalar(out=mk2[:], in0=l2[:], scalar1=st1[:, 1:2], scalar2=None, op0=ALU.is_equal),
                                 reads=[B_l2, B_st], writes=[B_mk2])
                            S.op("dve", lambda e: e.tensor_tensor(out=st1[:, 2:3], in0=st1[:, 1:2], in1=st1[:, 0:1], op=ALU.subtract), reads=[B_st], writes=[B_st])
                            S.op("act", lambda e: e.activation(out=st1[:, 3:4], in_=st1[:, 2:3], func=AF.Exp), reads=[B_st], writes=[B_st])
                            S.op("dve", lambda e: e.tensor_scalar(out=st1[:, 4:5], in0=st1[:, 3:4], scalar1=1.0, scalar2=None, op0=ALU.add), reads=[B_st], writes=[B_st])
                            S.op("dve", lambda e: e.reciprocal(out=st1[:, 5:6], in_=st1[:, 4:5]), reads=[B_st], writes=[B_st])
                            S.op("dve", lambda e: e.tensor_tensor(out=st1[:, 6:7], in0=st1[:, 3:4], in1=st1[:, 5:6], op=ALU.mult), reads=[B_st], writes=[B_st])
                            if not SPARSE_MOE:
                                S.op("dve", lambda e: e.tensor_scalar(out=comb[:], in0=mk1[:], scalar1=st1[:, 5:6], scalar2=None, op0=ALU.mult),
                                     reads=[B_mk1, B_st], writes=[B_comb])
                                S.op("dve", lambda e: e.scalar_tensor_tensor(out=comb[:], in0=mk2[:], scalar=st1[:, 6:7], in1=comb[:], op0=ALU.mult, op1=ALU.add),
                                     reads=[B_mk2, B_st, B_comb], writes=[B_comb])
                                S.op("pe", lambda e: e.matmul(ps[1][0:8, 0:128], comb[:], ident[:], start=True, stop=True),
                                     sig=True, reads=[B_comb, B_const], writes=[B_ps[1]])
                                S.op("act", lambda e: e.activation(out=combT[:, tt * 128:(tt + 1) * 128], in_=ps[1][0:8, 0:128], func=AF.Copy),
                                     reads=[B_ps[1]], writes=[B_combT])
                                continue
                            T_ = c0 // 128 + tt
                            S.op("dve", lambda e: e.tensor_copy(out=mk1s[:, T_, :], in_=mk1[:]), reads=[B_mk1], writes=[B_mks])
                            S.op("dve", lambda e: e.tensor_copy(out=mk2s[:, T_, :], in_=mk2[:]), reads=[B_mk2], writes=[B_mks])
                            S.op("dve", lambda e: e.tensor_tensor(out=sel[:], in0=mk1[:], in1=mk2[:], op=ALU.add), reads=[B_mk1, B_mk2], writes=[B_sel])
                            ft = first_tile[0]
                            S.op("pe", lambda e: e.matmul(ps[2][:, 0:8], triu[:], sel[:], start=True, stop=ft), sig=ft, reads=[B_sel, B_const], writes=[B_ps[2]])
                            if not ft:
                                S.op("pe", lambda e: e.matmul(ps[2][:, 0:8], ones_f[:], selsum[:], start=False, stop=True),
                                     sig=True, reads=[B_selsum, B_const], writes=[B_ps[2]])
                            S.op("dve", lambda e: e.tensor_tensor(out=tmp8[:, 0, :], in0=ps[2][:, 0:8], in1=mk1[:], op=ALU.mult), reads=[B_ps[2], B_mk1], writes=[B_tmp8])
                            S.op("dve", lambda e: e.tensor_tensor(out=tmp8[:, 1, :], in0=ps[2][:, 0:8], in1=mk2[:], op=ALU.mult), reads=[B_ps[2], B_mk2], writes=[B_tmp8])
                            S.op("dve", lambda e: e.reduce_sum(out=rtab[:, 0, T_:T_ + 1], in_=tmp8[:, 0, :], axis=mybir.AxisListType.X), reads=[B_tmp8], writes=[B_rtab])
                            S.op("dve", lambda e: e.reduce_sum(out=rtab[:, 1, T_:T_ + 1], in_=tmp8[:, 1, :], axis=mybir.AxisListType.X), reads=[B_tmp8], writes=[B_rtab])
                            if ft:
                                S.op("dve", lambda e: e.tensor_copy(out=selsum[:], in_=sel[:]), reads=[B_sel], writes=[B_selsum])
                            else:
                                S.op("dve", lambda e: e.tensor_tensor(out=selsum[:], in0=selsum[:], in1=sel[:], op=ALU.add), reads=[B_sel, B_selsum], writes=[B_selsum])
                            first_tile[0] = False
                            S.op("dve", lambda e: e.tensor_copy(out=rtab[:, 2, T_:T_ + 1], in_=st1[:, 5:6]), reads=[B_st], writes=[B_rtab])
                            S.op("dve", lambda e: e.tensor_copy(out=rtab[:, 3, T_:T_ + 1], in_=st1[:, 6:7]), reads=[B_st], writes=[B_rtab])
                            for k in range(8):
                                bank = 3 + k // 4
                                S.op("pe", lambda e: e.matmul(ps[bank][:, (k % 4) * 128:(k % 4 + 1) * 128], h2s[:, k, tt * 128:(tt + 1) * 128], ident_bf[:],
                                                              start=True, stop=True), sig=True, reads=[B_h2s, B_const], writes=[B_ps[bank]])
                            S.op("act", lambda e: e.activation(out=h2rows[:, T_, 0:512], in_=ps[3][:, 0:512], func=AF.Copy), reads=[B_ps[3]], writes=[B_h2rows])
                            S.op("dve", lambda e: e.tensor_copy(out=h2rows[:, T_, 512:1024], in_=ps[4][:, 0:512]), reads=[B_ps[4]], writes=[B_h2rows])
                        if not SPARSE_MOE:
                            S.dma("sp", CBd[:, c0:c0 + N], combT[:, 0:N], reads=[B_combT], writes=[B_CBd])
                if moe and SPARSE_MOE:
                    X_ = mybir.AxisListType.X
                    def dv(fn, extra_r=(), extra_w=()):
                        S.op("dve", fn, reads=[B_rt] + list(extra_r), writes=[B_rt] + list(extra_w))
                    cntv, accv, padv, endv, basev = (rt[:, j, 0:8] for j in range(5))
                    tef, bgu, bdn = rt[:, 5, 0:NTILE], rt[:, 6, 0:NTILE], rt[:, 7, 0:NTILE]
                    S.op("pe", lambda e: e.matmul(ps[2][:, 0:8], ones_f[:], selsum[:], start=True, stop=True), sig=True, reads=[B_selsum, B_const], writes=[B_ps[2]])
                    dv(lambda e: e.tensor_copy(out=cntv, in_=ps[2][:, 0:8]), extra_r=[B_ps[2]])
                    dv(lambda e: e.tensor_scalar(out=accv, in0=cntv, scalar1=0.0, scalar2=None, op0=ALU.is_gt))
                    for j in range(1, 8):
                        dv(lambda e: e.scalar_tensor_tensor(out=accv, in0=cntv, scalar=float(TS * j), in1=accv, op0=ALU.is_gt, op1=ALU.add))
                    dv(lambda e: e.tensor_scalar(out=padv, in0=accv, scalar1=float(TS), scalar2=None, op0=ALU.mult))
                    dv(lambda e: e.tensor_copy(out=endv[:, 0:1], in_=padv[:, 0:1]))
                    for ee in range(1, 8):
                        dv(lambda e: e.tensor_tensor(out=endv[:, ee:ee + 1], in0=endv[:, ee - 1:ee], in1=padv[:, ee:ee + 1], op=ALU.add))
                    dv(lambda e: e.memset(basev[:, 0:1], 0.0))
                    dv(lambda e: e.tensor_copy(out=basev[:, 1:8], in_=endv[:, 0:7]))
                    for t_ in range(NTILE):
                        dv(lambda e: e.tensor_scalar(out=tmp8[:, 0, 0:7], in0=endv[:, 0:7], scalar1=float(TS * t_), scalar2=None, op0=ALU.is_le), extra_w=[B_tmp8])
                        dv(lambda e: e.reduce_sum(out=tef[:, t_:t_ + 1], in_=tmp8[:, 0, 0:7], axis=X_), extra_r=[B_tmp8])
                    for q in range(2):
                        mks_ = mk1s if q == 0 else mk2s
                        sf_ = rt[:, q, 8:8 + 24]
                        for ee in range(8):
                            S.op("dve", lambda e: e.scalar_tensor_tensor(out=rtab[:, q, :], in0=mks_[:, :, ee], scalar=basev[:, ee:ee + 1], in1=rtab[:, q, :],
                                                                         op0=ALU.mult, op1=ALU.add), reads=[B_mks, B_rt, B_rtab], writes=[B_rtab])
                        S.op("dve", lambda e: e.tensor_copy(out=sloti[:, q, :], in_=rtab[:, q, :]), reads=[B_rtab], writes=[B_sloti])
                    io = vecs[:, IOTA_COL:IOTA_COL + 1]
                    dv(lambda e: e.tensor_scalar(out=bgu, in0=tef, scalar1=2048.0, scalar2=io, op0=ALU.mult, op1=ALU.add), extra_r=[B_vecs])
                    dv(lambda e: e.tensor_scalar(out=bdn, in0=tef, scalar1=float(DFF), scalar2=io, op0=ALU.mult, op1=ALU.add), extra_r=[B_vecs])
                    for hk in range(16):
                        S.op("dve", lambda e: e.tensor_scalar(out=widx_gu[:, :, hk], in0=bgu, scalar1=float((hk // 8) * 1024 + (hk % 8) * 128), scalar2=None, op0=ALU.add),
                             reads=[B_rt], writes=[B_widx])
                    for f_ in range(22):
                        S.op("dve", lambda e: e.tensor_scalar(out=widx_d[:, :, f_], in0=bdn, scalar1=float(f_ * 128), scalar2=None, op0=ALU.add),
                             reads=[B_rt], writes=[B_widx])
                    if dbg:
                        d_rt = nc.dram_tensor("dbg_rt", [128, 8, 32], F32, kind="ExternalOutput").ap()
                        B_drt = S.buf("dbg_rt", True)
                        S.dma("sp", d_rt, rt[:], reads=[B_rt], writes=[B_drt])
                    for T_ in range(32):
                        for q in range(2):
                            S.dma_fn("pool", lambda e: e.indirect_dma_start(out=XS[:, :], out_offset=bass.IndirectOffsetOnAxis(ap=sloti[:, q, T_:T_ + 1], axis=0),
                                                                            in_=h2rows[:, T_, :], in_offset=None),
                                     reads=[B_h2rows, B_sloti], writes=[B_XS])
                S.barrier()
                S.stop(f"{i}:C1")
            if moe and SPARSE_MOE:
                with ExitStack() as pc2:
                    NH = 11
                    wg = [sb(pc2, f"wg{b}", [128, 8, NH * 128], BF16) for b in range(2)]
                    wu = [sb(pc2, f"wu{b}", [128, 8, NH * 128], BF16) for b in range(2)]
                    wd = [sb(pc2, f"wd{b}", [128, NH, 1024], BF16) for b in range(2)]
                    B_w = [S.buf(f"wffn{b}", True) for b in range(2)]
                    xs = sb(pc2, "xs", [128, 4, 1024], BF16); B_xs_ = S.buf("xs", True)
                    xT = sb(pc2, "xT", [128, 8, 512], BF16); B_xT = S.buf("xT")
                    act = sb(pc2, "act", [128, NH, 512], BF16); B_act = S.buf("act")
                    sg = [sb(pc2, f"sg{b}", [128, 512], F32) for b in range(2)]; B_sg = [S.buf(f"sg{b}") for b in range(2)]
                    yacc = sb(pc2, "yacc", [128, 4, 1024], F32); B_yacc = S.buf("yacc")

                    def load_xs(t_):
                        S.dma("sp", xs[:], XS[t_ * TS:(t_ + 1) * TS, :].rearrange("(a s) d -> s a d", s=128), reads=[B_XS], writes=[B_xs_])
                    load_xs(0)
                    npass = 0
                    for t_ in range(NTILE):
                        for half in range(2):
                            b = npass % 2
                            for k in range(8):
                                S.dma_fn("pool", lambda e: e.indirect_dma_start(out=wg[b][:, k, :], out_offset=None, in_=WgH[:, :],
                                                                                in_offset=bass.IndirectOffsetOnAxis(ap=widx_gu[:, t_, half * 8 + k:half * 8 + k + 1], axis=0)),
                                         reads=[B_widx], writes=[B_w[b]])
                                S.dma_fn("pool", lambda e: e.indirect_dma_start(out=wu[b][:, k, :], out_offset=None, in_=WuH[:, :],
                                                                                in_offset=bass.IndirectOffsetOnAxis(ap=widx_gu[:, t_, half * 8 + k:half * 8 + k + 1], axis=0)),
                                         reads=[B_widx], writes=[B_w[b]])
                            for f in range(NH):
                                S.dma_fn("pool", lambda e: e.indirect_dma_start(out=wd[b][:, f, :], out_offset=None, in_=WdF[:, :],
                                                                                in_offset=bass.IndirectOffsetOnAxis(ap=widx_d[:, t_, half * NH + f:half * NH + f + 1], axis=0)),
                                         reads=[B_widx], writes=[B_w[b]])
                            if half == 0:
                                for k in range(8):
                                    bank = 4 + (k % 2)
                                    for a in range(4):
                                        S.op("pe", lambda e: e.matmul(ps[bank][:, a * 128:(a + 1) * 128], xs[:, a, k * 128:(k + 1) * 128], ident_bf[:], start=True, stop=True),
                                             sig=True, reads=[B_xs_, B_const], writes=[B_ps[bank]])
                                    if k % 2 == 0:
                                        S.op("act", lambda e: e.activation(out=xT[:, k, :], in_=ps[bank][:], func=AF.Copy), reads=[B_ps[bank]], writes=[B_xT])
                                    else:
                                        S.op("dve", lambda e: e.tensor_copy(out=xT[:, k, :], in_=ps[bank][:]), reads=[B_ps[bank]], writes=[B_xT])
                            else:
                                if t_ + 1 < NTILE:
                                    load_xs(t_ + 1)
                            for f in range(NH):
                                gb, ub = 0 + (f % 2), 2 + (f % 2)
                                for k in range(8):
                                    S.op("pe", lambda e: e.matmul(ps[gb][:], wg[b][:, k, f * 128:(f + 1) * 128], xT[:, k, :], start=(k == 0), stop=(k == 7)),
                                         sig=(k == 7), reads=[B_w[b], B_xT], writes=[B_ps[gb]])
                                for k in range(8):
                                    S.op("pe", lambda e: e.matmul(ps[ub][:], wu[b][:, k, f * 128:(f + 1) * 128], xT[:, k, :], start=(k == 0), stop=(k == 7)),
                                         sig=(k == 7), reads=[B_w[b], B_xT], writes=[B_ps[ub]])
                                S.op("act", lambda e: e.activation(out=sg[f % 2][:], in_=ps[gb][:], func=AF.Silu), reads=[B_ps[gb]], writes=[B_sg[f % 2]])
                                S.op("dve", lambda e: e.tensor_tensor(out=act[:, f, :], in0=ps[ub][:], in1=sg[f % 2][:], op=ALU.mult),
                                     reads=[B_ps[ub], B_sg[f % 2]], writes=[B_act])
                            cntd = 0
                            for a in range(4):
                                for dh in range(2):
                                    ob = 4 + (cntd % 4)
                                    cntd += 1
                                    for f in range(NH):
                                        S.op("pe", lambda e: e.matmul(ps[ob][:], act[:, f, a * 128:(a + 1) * 128], wd[b][:, f, dh * 512:(dh + 1) * 512],
                                                                      start=(f == 0), stop=(f == NH - 1)), sig=(f == NH - 1), reads=[B_w[b], B_act], writes=[B_ps[ob]])
                                    if half == 0:
                                        S.op("act", lambda e: e.activation(out=yacc[:, a, dh * 512:(dh + 1) * 512], in_=ps[ob][:], func=AF.Copy),
                                             reads=[B_ps[ob]], writes=[B_yacc])
                                    else:
                                        S.op("dve", lambda e: e.tensor_tensor(out=yacc[:, a, dh * 512:(dh + 1) * 512], in0=ps[ob][:], in1=yacc[:, a, dh * 512:(dh + 1) * 512], op=ALU.add),
                                             reads=[B_ps[ob], B_yacc], writes=[B_yacc])
                            if half == 1:
                                S.dma("sp", YS[t_ * TS:(t_ + 1) * TS, :].rearrange("(a s) d -> s a d", s=128), yacc[:], reads=[B_yacc], writes=[B_YS])
                            npass += 1
                    S.barrier()
                    S.stop(f"{i}:C2s")
                with ExitStack() as pg:
                    yg = [[sb(pg, f"yg{q}{b}", [128, 1024], F32) for b in range(2)] for q in range(2)]
                    B_yg = [[S.buf(f"yg{q}{b}", True) for b in range(2)] for q in range(2)]
                    fr = [sb(pg, f"fr{b}", [128, 1024], F32) for b in range(2)]; B_fr = [S.buf(f"fr{b}") for b in range(2)]
                    fTc = sb(pg, "fTc", [128, 8, 512], F32); B_fTck = [S.buf(f"fTc{k}") for k in range(8)]
                    fsq = sb(pg, "fsqg", [128, 8, 512], BF16); B_fsq = S.buf("fsqg")
                    xc3 = sb(pg, "xc3g", [128, 8, 512], F32); B_xc3 = S.buf("xc3g", True)
                    rs3 = sb(pg, "rs3g", [128, 512], F32); B_rs3 = S.buf("rs3g")
                    tm3 = sb(pg, "tm3g", [128, 512], F32); B_tm3 = S.buf("tm3g")
                    xo3 = sb(pg, "xo3g", [128, 8, 512], F32); B_xo3 = S.buf("xo3g")
                    for c in range(8):
                        S.dma("sp", xc3[:], xview(x_mid)[:, :, c * 512:(c + 1) * 512], reads=[B_xm], writes=[B_xc3])
                        for tt in range(4):
                            T_ = c * 4 + tt
                            b = T_ % 2
                            for q in range(2):
                                S.dma_fn("pool", lambda e: e.indirect_dma_start(out=yg[q][b][:, :], out_offset=None, in_=YS[:, :],
                                                                                in_offset=bass.IndirectOffsetOnAxis(ap=sloti[:, q, T_:T_ + 1], axis=0)),
                                         reads=[B_YS, B_sloti], writes=[B_yg[q][b]])
                            S.op("dve", lambda e: e.tensor_scalar(out=fr[b][:], in0=yg[0][b][:], scalar1=rtab[:, 2, T_:T_ + 1], scalar2=None, op0=ALU.mult),
                                 reads=[B_yg[0][b], B_rtab], writes=[B_fr[b]])
                            S.op("dve", lambda e: e.scalar_tensor_tensor(out=fr[b][:], in0=yg[1][b][:], scalar=rtab[:, 3, T_:T_ + 1], in1=fr[b][:], op0=ALU.mult, op1=ALU.add),
                                 reads=[B_yg[1][b], B_rtab, B_fr[b]], writes=[B_fr[b]])
                            for k in range(8):
                                S.op("pe", lambda e: e.matmul(ps[k][:, tt * 128:(tt + 1) * 128], fr[b][:, k * 128:(k + 1) * 128], ident[:], start=True, stop=True),
                                     sig=True, reads=[B_fr[b], B_const], writes=[B_ps[k]])
                        for k in range(8):
                            S.op("act", lambda e: e.activation(out=fTc[:, k, :], in_=ps[k][:], func=AF.Copy), reads=[B_ps[k]], writes=[B_fTck[k]])
                            S.op("act", lambda e: e.activation(out=fsq[:, k, :], in_=ps[k][:], func=AF.Square), reads=[B_ps[k]], writes=[B_fsq])
                        for k in range(8):
                            S.op("pe", lambda e: e.matmul(ps[0][:], ones_bf[:], fsq[:, k, :], start=(k == 0), stop=(k == 7)),
                                 sig=(k == 7), reads=[B_fsq, B_const], writes=[B_ps[0]])
                        rstd_from_ss(ps[0][:], B_ps[0], D, rs3[:], B_rs3, tm3[:], B_tm3)
                        for k in range(8):
                            S.op("dve", lambda e: e.scalar_tensor_tensor(out=fTc[:, k, :], in0=fTc[:, k, :], scalar=G_f[:, k, 0:1], in1=rs3[:],
                                                                         op0=ALU.mult, op1=ALU.mult), reads=[B_fTck[k], B_rs3, B_mod], writes=[B_fTck[k]])
                            S.op("pool", lambda e: e.tensor_tensor(out=xo3[:, k, :], in0=fTc[:, k, :], in1=xc3[:, k, :], op=ALU.add),
                                 reads=[B_fTck[k], B_xc3], writes=[B_xo3])
                        S.dma("sp", xview(x_dst)[:, :, c * 512:(c + 1) * 512], xo3[:], reads=[B_xo3], writes=[B_xd])
                    S.barrier()
                    S.stop(f"{i}:C2b")
            else:
                with ExitStack() as pc2:
                    NH = 11
                    wg = [sb(pc2, f"wg{b}", [128, 8, NH * 128], BF16) for b in range(2)]
                    wu = [sb(pc2, f"wu{b}", [128, 8, NH * 128], BF16) for b in range(2)]
                    wd = [sb(pc2, f"wd{b}", [128, NH, 1024], BF16) for b in range(2)]
                    B_w = [S.buf(f"wffn{b}", True) for b in range(2)]
                    h2c = [sb(pc2, f"h2c{b}", [128, 8, 512], BF16) for b in range(2)]
                    B_h2c = [S.buf(f"h2c{b}", True) for b in range(2)]
                    act = sb(pc2, "act", [128, NH, 512], BF16); B_act = S.buf("act")
                    sg = [sb(pc2, f"sg{b}", [128, 512], F32) for b in range(2)]; B_sg = [S.buf(f"sg{b}") for b in range(2)]
                    facc = sb(pc2, "facc", [128, 8, 512], F32); B_facc = S.buf("facc", True)
                    cbc = sb(pc2, "cbc", [128, 512], F32); B_cbc = S.buf("cbc", True)
                    otmp = [sb(pc2, f"otmp{b}", [128, 512], F32) for b in range(2)]; B_otmp = [S.buf(f"otmp{b}") for b in range(2)]
                    npass = 0
                    for ex in range(n_exp):
                        if moe:
                            Wg, Wu, Wd = W["moe_w_gate"][0, ex], W["moe_w_up"][0, ex], W["moe_w_down"][0, ex]
                        else:
                            Wg, Wu, Wd = W["ffn_w_gate"][0], W["ffn_w_up"][0], W["ffn_w_down"][0]
                        Wgv = Wg.rearrange("(k p) n -> p k n", p=128)
                        Wuv = Wu.rearrange("(k p) n -> p k n", p=128)
                        Wdv = Wd.rearrange("(f p) n -> p f n", p=128)
                        for half in range(2):
                            b = npass % 2
                            f0 = half * NH
                            for k in range(8):
                                S.dma("pool", wg[b][:, k, :], Wgv[:, k, f0 * 128:(f0 + NH) * 128], writes=[B_w[b]])
                                S.dma("pool", wu[b][:, k, :], Wuv[:, k, f0 * 128:(f0 + NH) * 128], writes=[B_w[b]])
                            for f in range(NH):
                                S.dma("pool", wd[b][:, f, :], Wdv[:, f0 + f, :], writes=[B_w[b]])
                            def load_h2c(cj):
                                c0j, Nj, _ = ffn_chunks[cj]
                                S.dma("sp", h2c[cj % 2][:, :, 0:Nj], H2T[:, :, c0j:c0j + Nj].rearrange("k p n -> p k n"), reads=[B_H2T], writes=[B_h2c[cj % 2]])
                            load_h2c(0)
                            for ci, (c0, N, isc) in enumerate(ffn_chunks):
                                hb = ci % 2
                                if ci + 1 < len(ffn_chunks):
                                    load_h2c(ci + 1)
                                if npass > 0:
                                    S.dma("sp", facc[:, :, 0:N], FT[:, :, c0:c0 + N].rearrange("k p n -> p k n"), reads=[B_FT], writes=[B_facc])
                                if moe:
                                    cb_src = bass.AP(CBd.tensor, ex * NTOK + c0, [[0, 128], [1, N]])
                                    S.dma("sp", cbc[:, 0:N], cb_src, reads=[B_CBd], writes=[B_cbc])
                                for f in range(NH):
                                    gb, ub = 0 + (f % 2), 2 + (f % 2)
                                    for k in range(8):
                                        S.op("pe", lambda e: e.matmul(ps[gb][:, 0:N], wg[b][:, k, f * 128:(f + 1) * 128], h2c[hb][:, k, 0:N],
                                                                      start=(k == 0), stop=(k == 7)), sig=(k == 7), reads=[B_w[b], B_h2c[hb]], writes=[B_ps[gb]])
                                    for k in range(8):
                                        S.op("pe", lambda e: e.matmul(ps[ub][:, 0:N], wu[b][:, k, f * 128:(f + 1) * 128], h2c[hb][:, k, 0:N],
                                                                      start=(k == 0), stop=(k == 7)), sig=(k == 7), reads=[B_w[b], B_h2c[hb]], writes=[B_ps[ub]])
                                    S.op("act", lambda e: e.activation(out=sg[f % 2][:, 0:N], in_=ps[gb][:, 0:N], func=AF.Silu), reads=[B_ps[gb]], writes=[B_sg[f % 2]])
                                    S.op("dve", lambda e: e.tensor_tensor(out=act[:, f, 0:N], in0=ps[ub][:, 0:N], in1=sg[f % 2][:, 0:N], op=ALU.mult),
                                         reads=[B_ps[ub], B_sg[f % 2]], writes=[B_act])
                                for j in range(8):
                                    ob = 4 + (j % 4)
                                    for f in range(NH):
                                        S.op("pe", lambda e: e.matmul(ps[ob][:, 0:N], wd[b][:, f, j * 128:(j + 1) * 128], act[:, f, 0:N],
                                                                      start=(f == 0), stop=(f == NH - 1)), sig=(f == NH - 1), reads=[B_w[b], B_act], writes=[B_ps[ob]])
                                    if moe:
                                        S.op("dve", lambda e: e.tensor_tensor(out=otmp[j % 2][:, 0:N], in0=ps[ob][:, 0:N], in1=cbc[:, 0:N], op=ALU.mult),
                                             reads=[B_ps[ob], B_cbc], writes=[B_otmp[j % 2]])
                                        if npass == 0:
                                            S.op("pool", lambda e: e.tensor_copy(out=facc[:, j, 0:N], in_=otmp[j % 2][:, 0:N]), reads=[B_otmp[j % 2]], writes=[B_facc])
                                        else:
                                            S.op("pool", lambda e: e.tensor_tensor(out=facc[:, j, 0:N], in0=facc[:, j, 0:N], in1=otmp[j % 2][:, 0:N], op=ALU.add),
                                                 reads=[B_otmp[j % 2], B_facc], writes=[B_facc])
                                    else:
                                        if npass == 0:
                                            S.op("act", lambda e: e.activation(out=facc[:, j, 0:N], in_=ps[ob][:, 0:N], func=AF.Copy), reads=[B_ps[ob]], writes=[B_facc])
                                        else:
                                            S.op("dve", lambda e: e.tensor_tensor(out=facc[:, j, 0:N], in0=ps[ob][:, 0:N], in1=facc[:, j, 0:N], op=ALU.add),
                                                 reads=[B_ps[ob], B_facc], writes=[B_facc])
                                S.dma("sp", FT[:, :, c0:c0 + N].rearrange("k p n -> p k n"), facc[:, :, 0:N], reads=[B_facc], writes=[B_FT])
                            npass += 1
                    S.barrier()
                    S.stop(f"{i}:C2")
            with ExitStack() as pc3:
                fc = sb(pc3, "fc", [128, 8, 512], F32); B_fc = S.buf("fc", True)
                fsq = sb(pc3, "fsq", [128, 8, 512], BF16); B_fsq = S.buf("fsq")
                xc3 = sb(pc3, "xc3", [128, 8, 512], F32); B_xc3 = S.buf("xc3", True)
                rs3 = sb(pc3, "rs3", [128, 512], F32); B_rs3 = S.buf("rs3")
                tm3 = sb(pc3, "tm3", [128, 512], F32); B_tm3 = S.buf("tm3")
                xo3 = sb(pc3, "xo3", [128, 8, 512], F32); B_xo3 = S.buf("xo3")
                nxt_mod = (i + 1 < n_layers)
                if nxt_mod:
                    wm_n, B_wm_n = mod_alloc(pc3)
                    mod_gs = list(range(12))
                for ci, (c0, N, isc) in enumerate([] if (moe and SPARSE_MOE) else ffn_chunks):
                    if nxt_mod and mod_gs:
                        mod_group(i + 1, mod_gs.pop(0), wm_n, B_wm_n)
                        if ci % 2 == 1 and mod_gs:
                            mod_group(i + 1, mod_gs.pop(0), wm_n, B_wm_n)
                    s = 1 if isc else 0
                    cs = 0 if isc else c0
                    S.dma("sp", fc[:, :, 0:N], FT[:, :, c0:c0 + N].rearrange("k p n -> p k n"), reads=[B_FT], writes=[B_fc])
                    S.dma("sp", xc3[:, :, 0:N], xview(y_mid if isc else x_mid)[:, :, cs:cs + N], reads=[B_ym if isc else B_xm], writes=[B_xc3])
                    for k in range(8):
                        S.op("act", lambda e: e.activation(out=fsq[:, k, 0:N], in_=fc[:, k, 0:N], func=AF.Square), reads=[B_fc], writes=[B_fsq])
                    for k in range(8):
                        S.op("pe", lambda e: e.matmul(ps[0][:, 0:N], ones_bf[:], fsq[:, k, 0:N], start=(k == 0), stop=(k == 7)),
                             sig=(k == 7), reads=[B_fsq, B_const], writes=[B_ps[0]])
                    rstd_from_ss(ps[0][:, 0:N], B_ps[0], D, rs3[:, 0:N], B_rs3, tm3[:, 0:N], B_tm3)
                    for k in range(8):
                        S.op("dve", lambda e: e.scalar_tensor_tensor(out=fc[:, k, 0:N], in0=fc[:, k, 0:N], scalar=G_f[:, k, s:s + 1], in1=rs3[:, 0:N],
                                                                     op0=ALU.mult, op1=ALU.mult), reads=[B_fc, B_rs3, B_mod], writes=[B_fc])
                        S.op("pool", lambda e: e.tensor_tensor(out=xo3[:, k, 0:N], in0=fc[:, k, 0:N], in1=xc3[:, k, 0:N], op=ALU.add),
                             reads=[B_fc, B_xc3], writes=[B_xo3])
                    S.dma("sp", xview(y_dst if isc else x_dst)[:, :, cs:cs + N], xo3[:, :, 0:N], reads=[B_xo3], writes=[B_yd if isc else B_xd])
                if nxt_mod:
                    while mod_gs:
                        mod_group(i + 1, mod_gs.pop(0), wm_n, B_wm_n)
                    mod_finish(i + 1)
                S.barrier()
            x_src, B_xs = x_dst, B_xd
            y_src, B_ys = y_dst, B_yd
        S.dead = False
        if dbg:
            d1 = nc.dram_tensor("dbg_sloti", [128, 2, 32], I32, kind="ExternalOutput").ap()
            d2 = nc.dram_tensor("dbg_rtab", [128, 4, 32], F32, kind="ExternalOutput").ap()
            d3 = nc.dram_tensor("dbg_widx_gu", [128, NTILE, 16], I32, kind="ExternalOutput").ap()
            d4 = nc.dram_tensor("dbg_widx_d", [128, NTILE, 22], I32, kind="ExternalOutput").ap()
            B_dd = S.buf("dbg_dd", True)
            S.dma("sp", d1, sloti[:], reads=[B_sloti], writes=[B_dd])
            S.dma("sp", d2, rtab[:], reads=[B_rtab], writes=[B_dd])
            S.dma("sp", d3, widx_gu[:], reads=[B_widx], writes=[B_dd])
            S.dma("sp", d4, widx_d[:], reads=[B_widx], writes=[B_dd])
        if n_layers < DEPTH or stop_after is not None:
            with ExitStack() as pd:
                t = sb(pd, "dcp", [128, 8, 512], F32); B_t = S.buf("dcp", True)
                for c in range(8):
                    S.dma("sp", t[:], xview(x_src)[:, :, c * 512:(c + 1) * 512], reads=[B_xs], writes=[B_t])
                    S.dma("sp", xview(outT)[:, :, c * 512:(c + 1) * 512], t[:], reads=[B_t], writes=[B_out])
        S.barrier()
        print(f"[kernel] instructions={S.n_inst} waits={S.n_wait} dsems={len(S.dsems)}")
    return nc


def _shared_inputs(inp):
    cos2, sin2, pm, ident, invt, triu = _const_tables()
    shared = {"cos2": cos2, "sin2": sin2, "pm": pm, "ident": ident, "invt": invt, "triu": triu}
    for nm in WEIGHT_NAMES:
        shared[nm] = np.ascontiguousarray(inp[nm], dtype=np.float32)
    if SPARSE_MOE:
        for nm in ("moe_w_gate", "moe_w_up"):
            w = shared.pop(nm)[0]
            shared[nm + "_h"] = np.ascontiguousarray(w.reshape(NE, D, 2, DFF // 2).transpose(0, 2, 1, 3)).reshape(NE * 2 * D, DFF // 2)
        shared["moe_w_down_f"] = np.ascontiguousarray(shared.pop("moe_w_down")[0]).reshape(NE * DFF, D)
    return shared


_PROGRAM = None


def kernel(**inputs):
    global _PROGRAM
    inp = {k: np.asarray(v) for k, v in inputs.items()}
    n = 8
    shared = _shared_inputs(inp)
    in_maps = []
    for b in range(n):
        m = dict(shared)
        m["xT"] = np.ascontiguousarray(inp["x"][b].T, dtype=np.float32)
        m["ctxT"] = np.ascontiguousarray(inp["ctx"][b].T, dtype=np.float32)
        m["vecs"] = _pack_vecs(inp, b)
        in_maps.append(m)
    if _PROGRAM is None:
        _PROGRAM = build_program()
    res = run_bass_kernel_spmd(_PROGRAM, in_maps, core_ids=list(range(n)))
    out = np.stack([np.ascontiguousarray(res.results[b]["outT"].T) for b in range(n)], axis=0)
    return out.astype(np.float32)
```

```python
import math
from contextlib import ExitStack
import numpy as np
import concourse.bass as bass
import concourse.mybir as mybir
from concourse.bass_utils import run_bass_kernel_spmd

F32 = mybir.dt.float32
BF16 = mybir.dt.bfloat16
AF = mybir.ActivationFunctionType
ALU = mybir.AluOpType

D = 1024
T = 4096
TC = 256
NTOK = T + TC
DEPTH = 2
DFF = 2816
NE = 8
EPS = 1e-6
ZW = 4384
ZL = 8
ZC = 4120
CHUNKS = [(c * 512, 512, False) for c in range(8)] + [(T, TC, True)]
TS = 512
NTILE = 23
NSLOT = NTILE * TS
I32 = mybir.dt.int32
import os
SPARSE_MOE = os.environ.get("KDENSE_MOE", "0") != "1"


class Eng:
    def __init__(self, name, eng, sem):
        self.name, self.eng, self.sem = name, eng, sem
        self.seq = 0
        self.sig_seq = []
        self.last = None
        self.last_sig = True
        self.waited = {}


class DSem:
    def __init__(self, sem):
        self.sem = sem
        self.total = 0


class Buf:
    def __init__(self, name, dsem=None):
        self.name = name
        self.dsem = dsem
        self.multi = False
        self.writers = {}
        self.readers = {}


class Sched:
    def __init__(self, nc, stack):
        self.nc = nc
        self.stack = stack
        self.engs = {}
        for nm, e in (("pe", nc.tensor), ("act", nc.scalar), ("dve", nc.vector),
                      ("pool", nc.gpsimd), ("sp", nc.sync)):
            sem = stack.enter_context(nc.semaphore("s_" + nm))
            self.engs[nm] = Eng(nm, e, sem)
        self.dsems = []
        self.dsem_by_name = {}
        self.n_wait = 0
        self.n_inst = 0
        self.dead = False
        self.stop_after = None

    def stop(self, tag):
        if self.stop_after == tag:
            self.dead = True

    def new_dsem(self, name):
        if name in self.dsem_by_name:
            return self.dsem_by_name[name]
        d = DSem(self.stack.enter_context(self.nc.semaphore("d_" + name)))
        self.dsems.append(d)
        self.dsem_by_name[name] = d
        return d

    def buf(self, name, dma=False):
        return Buf(name, name if dma else None)

    def _dsem_for(self, bufs, en):
        for b in bufs:
            if b.dsem is not None:
                return self.new_dsem(b.dsem + "_" + en)
        raise AssertionError("no dma-capable buf")

    def _signal_last(self, E):
        if not E.last_sig:
            E.last.then_inc(E.sem, 1)
            E.last_sig = True
            E.sig_seq.append(E.seq)

    def _resolve(self, tok):
        if tok[0] == "c":
            _, E, seq = tok
            ss = E.sig_seq
            if ss and ss[-1] >= seq:
                lo, hi = 0, len(ss) - 1
                while lo < hi:
                    mid = (lo + hi) // 2
                    if ss[mid] >= seq:
                        hi = mid
                    else:
                        lo = mid + 1
                return E.sem, lo + 1
            assert not E.last_sig and E.seq >= seq
            self._signal_last(E)
            return E.sem, len(ss)
        _, d, val = tok
        return d.sem, d.total

    def _wait(self, E, toks):
        need = {}
        for tok in toks:
            sem, val = self._resolve(tok)
            key = id(sem)
            if E.waited.get(key, 0) >= val:
                continue
            if key not in need or need[key][1] < val:
                need[key] = (sem, val)
        for key, (sem, val) in need.items():
            E.eng.wait_ge(sem, val)
            E.waited[key] = val
            self.n_wait += 1

    def op(self, en, fn, reads=(), writes=(), sig=None):
        if self.dead:
            return None
        E = self.engs[en]
        toks = []
        for b in reads:
            toks.extend(b.writers.values())
        for b in writes:
            for t in list(b.readers.values()) + list(b.writers.values()):
                if t[0] == "c" and t[1] is E:
                    continue
                toks.append(t)
        self._wait(E, toks)
        inst = fn(E.eng)
        E.seq += 1
        E.last = inst
        E.last_sig = False
        self.n_inst += 1
        tok = ("c", E, E.seq)
        for b in reads:
            b.readers[("c", en)] = tok
        for b in writes:
            b.writers = {("c", en): tok}
            b.readers = {}
        if sig or (sig is None and en != "pe"):
            self._signal_last(E)
        return inst

    def dma(self, en, out, in_, reads=(), writes=(), sem_buf=None, **kw):
        if self.dead:
            return None
        E = self.engs[en]
        d = self._dsem_for([sem_buf] if sem_buf is not None else list(writes) + list(reads), en)
        toks = []
        for b in reads:
            toks.extend(b.writers.values())
        for b in writes:
            toks.extend(b.readers.values())
            for t in b.writers.values():
                if t[0] == "d" and (t[1] is d or b.multi):
                    continue
                toks.append(t)
        self._wait(E, toks)
        inst = E.eng.dma_start(out=out, in_=in_, **kw)
        inst.then_inc(d.sem, 16)
        d.total += 16
        self.n_inst += 1
        tok = ("d", d, d.total)
        for b in reads:
            b.readers[("d", id(d))] = tok
        for b in writes:
            if b.multi:
                b.writers = {k: v for k, v in b.writers.items() if k[0] == "d"}
                b.writers[("d", id(d))] = tok
            else:
                b.writers = {("d", id(d)): tok}
            b.readers = {}
        return inst

    def dma_fn(self, en, fn, reads=(), writes=()):
        if self.dead:
            return None
        E = self.engs[en]
        d = self._dsem_for(list(writes) + list(reads), en)
        toks = []
        for b in reads:
            toks.extend(b.writers.values())
        for b in writes:
            toks.extend(b.readers.values())
            for t in b.writers.values():
                if t[0] == "d" and t[1] is d:
                    continue
                toks.append(t)
        self._wait(E, toks)
        inst = fn(E.eng)
        inst.then_inc(d.sem, 16)
        d.total += 16
        self.n_inst += 1
        tok = ("d", d, d.total)
        for b in reads:
            b.readers[("d", id(d))] = tok
        for b in writes:
            b.writers = {("d", id(d)): tok}
            b.readers = {}
        return inst

    def barrier(self):
        if self.dead:
            return
        for E in self.engs.values():
            if E.last is not None:
                self._signal_last(E)
        for E in self.engs.values():
            for E2 in self.engs.values():
                if E2.sig_seq:
                    val = len(E2.sig_seq)
                    if E.waited.get(id(E2.sem), 0) < val:
                        E.eng.wait_ge(E2.sem, val)
                        E.waited[id(E2.sem)] = val
            for d in self.dsems:
                if d.total and E.waited.get(id(d.sem), 0) < d.total:
                    E.eng.wait_ge(d.sem, d.total)
                    E.waited[id(d.sem)] = d.total


def _col(v):
    v = np.asarray(v, np.float32)
    return np.ascontiguousarray(v.reshape(-1, 128).T)


VEC_LAYER = 48 + 8 * 4 + 1 + 12 + 4 + 4
VEC_OFF = {"bmod": 0, "gpm": 48, "gqm": 56, "gpf": 64, "gqf": 72, "gsub": 80, "convw": 81,
           "pscale": 93, "lam": 97}
NV = 16 + DEPTH * VEC_LAYER + 64 + 1
IOTA_COL = NV - 1


def _voff(i, name):
    return 16 + i * VEC_LAYER + VEC_OFF[name]


def _pack_vecs(inp, b):
    v = np.zeros((128, NV), np.float32)
    v[:, 0:8] = _col(inp["c"][b])
    v[:, 8:16] = _col(inp["c_ctx"])
    for i in range(DEPTH):
        v[:, _voff(i, "bmod"):_voff(i, "bmod") + 48] = _col(inp["b_mod"][i])
        v[:, _voff(i, "gpm"):_voff(i, "gpm") + 8] = _col(inp["g_pre_mix"][i])
        v[:, _voff(i, "gqm"):_voff(i, "gqm") + 8] = _col(inp["g_post_mix"][i])
        v[:, _voff(i, "gpf"):_voff(i, "gpf") + 8] = _col(inp["g_pre_ffn"][i])
        v[:, _voff(i, "gqf"):_voff(i, "gqf") + 8] = _col(inp["g_post_ffn"][i])
        v[:, _voff(i, "gsub"):_voff(i, "gsub") + 1] = _col(inp["g_subln"][i])
        for r in range(3):
            v[:, _voff(i, "convw") + r * 4:_voff(i, "convw") + r * 4 + 4] = _col(inp["conv_w"][i][r])
        v[:, _voff(i, "pscale"):_voff(i, "pscale") + 4] = _col(inp["pool_scale"][i])
        for j, nm in enumerate(("lambda_q1", "lambda_k1", "lambda_q2", "lambda_k2")):
            v[0:64, _voff(i, "lam") + j] = np.asarray(inp[nm][i], np.float32)
    ro = 16 + DEPTH * VEC_LAYER
    rw = np.asarray(inp["router_w"][0], np.float32)
    v[:, ro:ro + 64] = rw.reshape(8, 128, 8).transpose(1, 0, 2).reshape(128, 64)
    v[:, IOTA_COL] = np.arange(128, dtype=np.float32)
    return v


def _const_tables():
    rows = np.repeat(np.arange(T // 64), 64).astype(np.float32)
    cols = np.tile(np.arange(64), T // 64).astype(np.float32)
    inv = (10000.0 ** (-np.arange(16, dtype=np.float32) / 16)).astype(np.float32)
    ang = np.stack([rows[:, None] * inv, cols[:, None] * inv], axis=1)
    ang = np.broadcast_to(ang[:, :, None, :], (T, 2, 2, 16)).reshape(T, 64)
    cos = np.cos(ang).astype(np.float32).T
    sin = np.sin(ang).astype(np.float32).T
    sgn = np.ones((64, 1), np.float32)
    pm = np.zeros((128, 128), np.float32)
    for m in range(128):
        dd = m % 64
        half = (dd % 32) // 16
        if half == 0:
            src = m + 16
            sgn[dd, 0] = -1.0
        else:
            src = m - 16
        pm[src, m] = 1.0
    sin = sin * sgn
    cos2 = np.ascontiguousarray(np.concatenate([cos, cos], 0))
    sin2 = np.ascontiguousarray(np.concatenate([sin, sin], 0))
    ident = np.eye(128, dtype=np.float32)
    triu = np.triu(np.ones((128, 128), np.float32), k=1)
    invt = np.ones((3, 4, 512), np.float32)
    for g, w in enumerate((2, 4, 8, 16)):
        def cnt(t, L):
            lo = np.clip(t - w // 2, 0, L)
            hi = np.clip(t - w // 2 + w, 0, L)
            return (hi - lo).astype(np.float32)
        invt[0, g] = 1.0 / cnt(np.arange(0, 512), T)
        invt[1, g] = 1.0 / cnt(np.arange(T - 512, T), T)
        invt[2, g, :TC] = 1.0 / cnt(np.arange(0, TC), TC)
    invt = np.ascontiguousarray(np.broadcast_to(invt[None], (128, 3, 4, 512)))
    return cos2, sin2, pm, ident, invt, triu


WEIGHT_NAMES = ["w_mod", "w_in", "w_branch", "w_out", "pool_w", "ffn_w_gate", "ffn_w_up",
                "ffn_w_down", "moe_w_gate", "moe_w_up", "moe_w_down"]
WEIGHT_SHAPES = {
    "w_mod": [DEPTH, D, 6 * D], "w_in": [DEPTH, D, 6656], "w_branch": [DEPTH, 3, 512, D],
    "w_out": [DEPTH, D, D], "pool_w": [DEPTH, 4, 128, 128], "ffn_w_gate": [1, D, DFF],
    "ffn_w_up": [1, D, DFF], "ffn_w_down": [1, DFF, D], "moe_w_gate": [1, NE, D, DFF],
    "moe_w_up": [1, NE, D, DFF], "moe_w_down": [1, NE, DFF, D],
}


def build_program(n_layers=DEPTH, dbg=False, stop_after=None):
    nc = bass.Bass("TRN2", target_bir_lowering=False)
    din = {}

    def inp(name, shape):
        din[name] = nc.dram_tensor(name, list(shape), F32, kind="ExternalInput").ap()
        return din[name]

    xT_in = inp("xT", [D, T])
    ctxT_in = inp("ctxT", [D, TC])
    vecs_in = inp("vecs", [128, NV])
    cos_in = inp("cos2", [128, T])
    sin_in = inp("sin2", [128, T])
    pm_in = inp("pm", [128, 128])
    ident_in = inp("ident", [128, 128])
    invt_in = inp("invt", [128, 3, 4, 512])
    triu_in = inp("triu", [128, 128])
    if SPARSE_MOE:
        W = {n: inp(n, WEIGHT_SHAPES[n]) for n in WEIGHT_NAMES if not n.startswith("moe_")}
        WgH = inp("moe_w_gate_h", [NE * 2 * D, DFF // 2])
        WuH = inp("moe_w_up_h", [NE * 2 * D, DFF // 2])
        WdF = inp("moe_w_down_f", [NE * DFF, D])
    else:
        W = {n: inp(n, WEIGHT_SHAPES[n]) for n in WEIGHT_NAMES}
    outT = nc.dram_tensor("outT", [D, T], F32, kind="ExternalOutput").ap()

    def scratch(name, shape, dt):
        return nc.dram_tensor(name, list(shape), dt, kind="Internal").ap()

    xA = scratch("xA", [D, T], F32)
    xB = scratch("xB", [D, T], F32)
    yA = scratch("yA", [D, TC], F32)
    yB = scratch("yB", [D, TC], F32)
    ZT = scratch("ZT", [40, 128, ZW], BF16)
    QT = scratch("QT", [4, 128, NTOK], BF16)
    AOT = scratch("AOT", [4, 128, NTOK], BF16)
    H2T = scratch("H2T", [8, 128, NTOK], BF16)
    FT = scratch("FT", [8, 128, NTOK], F32)
    CBd = scratch("CBd", [NE, NTOK], F32)
    XS = scratch("XS", [NSLOT, D], BF16)
    YS = scratch("YS", [NSLOT, D], F32)

    with ExitStack() as st:
        S = Sched(nc, st)
        S.stop_after = stop_after

        uniq = [0]

        def sb(stack, name, shape, dt):
            uniq[0] += 1
            return stack.enter_context(nc.sbuf_tensor(f"sb{uniq[0]}_{name}", list(shape), dt))

        B_xA, B_xB, B_yA, B_yB = (S.buf(n, True) for n in ("xA", "xB", "yA", "yB"))
        B_ZT, B_QT, B_H2T, B_FT, B_CBd, B_out, B_AOT = (S.buf(n, True) for n in ("ZT", "QT", "H2T", "FT", "CBd", "out", "AOT"))
        B_XS, B_YS = S.buf("XS", True), S.buf("YS", True)
        B_ZT.multi = True
        B_QT.multi = True
        B_xin = S.buf("xin")
        B_cin = S.buf("cin")

        vecs = sb(st, "vecs", [128, NV], F32)
        B_vecs = S.buf("vecs", True)
        ones_bf = sb(st, "ones_bf", [128, 128], BF16)
        ones_f = sb(st, "ones_f", [128, 128], F32)
        ident = sb(st, "ident", [128, 128], F32)
        pm = sb(st, "pm", [128, 128], BF16)
        B_const = S.buf("const", True)
        zero_bf = sb(st, "zero_bf", [128, 16], BF16)
        ident_bf = sb(st, "ident_bf", [128, 128], BF16)
        triu = sb(st, "triu", [128, 128], F32)
        mk1s = sb(st, "mk1s", [128, 32, 8], F32)
        mk2s = sb(st, "mk2s", [128, 32, 8], F32)
        rtab = sb(st, "rtab", [128, 4, 32], F32)
        sloti = sb(st, "sloti", [128, 2, 32], I32)
        widx_gu = sb(st, "widx_gu", [128, NTILE, 16], I32)
        widx_d = sb(st, "widx_d", [128, NTILE, 22], I32)
        B_mks, B_rtab, B_sloti, B_widx = S.buf("mks"), S.buf("rtab"), S.buf("sloti"), S.buf("widx")
        silc = sb(st, "silc", [128, 8, 2], BF16)
        B_silc = S.buf("silc")
        mods = []
        for l_ in range(2):
            mods.append((sb(st, f"modT{l_}", [128, 48, 2], F32), sb(st, f"A_m{l_}", [128, 8, 2], F32), sb(st, f"A_f{l_}", [128, 8, 2], F32),
                         sb(st, f"G_m{l_}", [128, 8, 2], F32), sb(st, f"G_f{l_}", [128, 8, 2], F32),
                         sb(st, f"lamv{l_}", [128, 4], F32),
                         S.buf(f"mod{l_}"), S.buf(f"lam{l_}")))
        modT, A_m, A_f, G_m, G_f, lamv, B_mod, B_lam = mods[0]
        ps = [st.enter_context(nc.psum_tensor(f"ps{i}", [128, 512], F32)) for i in range(8)]
        B_ps = [S.buf(f"ps{i}") for i in range(8)]

        S.dma("sp", vecs[:], vecs_in, writes=[B_vecs])
        S.dma("sp", ident[:], ident_in, writes=[B_const])
        S.dma("pool", pm[:], pm_in, writes=[B_const])
        S.dma("pool", ident_bf[:], ident_in, writes=[B_const])
        S.dma("sp", triu[:], triu_in, writes=[B_const])
        S.op("dve", lambda e: e.memset(ones_bf[:], 1.0), writes=[B_const])
        S.op("dve", lambda e: e.memset(ones_f[:], 1.0), writes=[B_const])
        S.op("dve", lambda e: e.memset(zero_bf[:], 0.0), writes=[B_const])
        for j in range(40):
            S.dma("sp", ZT[j, :, 0:8], zero_bf[:, 0:8], reads=[B_const], writes=[B_ZT])
            S.dma("sp", ZT[j, :, ZL + T:ZC], zero_bf[:, 0:16], reads=[B_const], writes=[B_ZT])
            S.dma("sp", ZT[j, :, ZC + TC:ZW], zero_bf[:, 0:8], reads=[B_const], writes=[B_ZT])
        if SPARSE_MOE and n_layers > 1:
            with ExitStack() as zs:
                zrow = sb(zs, "zrow", [128, 1024], BF16)
                B_zrow = S.buf("zrow")
                S.op("pool", lambda e: e.memset(zrow[:], 0.0), writes=[B_zrow])
                for r_ in range(NSLOT // 128):
                    S.dma("sp", XS[r_ * 128:(r_ + 1) * 128, :], zrow[:], reads=[B_zrow], writes=[B_XS])
                S.barrier()
        S.op("act", lambda e: e.activation(out=silc[:, :, 0], in_=vecs[:, 0:8], func=AF.Silu), reads=[B_vecs], writes=[B_silc])
        S.op("act", lambda e: e.activation(out=silc[:, :, 1], in_=vecs[:, 8:16], func=AF.Silu), reads=[B_vecs], writes=[B_silc])

        S.stop("setup")

        def vcol(i, name, k=0):
            o = _voff(i, name) + k
            return vecs[:, o:o + 1]

        def xview(ap):
            return ap.rearrange("(k p) n -> p k n", p=128)

        def mod_alloc(ls):
            return ([sb(ls, f"wm{b}", [128, 8, 512], BF16) for b in range(2)], [S.buf(f"wm{b}", True) for b in range(2)])

        def mod_group(i, g, wm, B_wm):
            wv = W["w_mod"][i].rearrange("(k p) n -> p k n", p=128)
            b = g % 2
            S.dma("pool", wm[b][:], wv[:, :, g * 512:(g + 1) * 512], writes=[B_wm[b]])
            for jj in range(4):
                j = g * 4 + jj
                for k in range(8):
                    S.op("pe", lambda e: e.matmul(ps[5][:, j * 2:j * 2 + 2], wm[b][:, k, jj * 128:(jj + 1) * 128],
                                                  silc[:, k, :], start=(k == 0), stop=(k == 7)),
                         sig=(k == 7), reads=[B_wm[b], B_silc], writes=[B_ps[5]])

        def compute_mod(i):
            with ExitStack() as ls:
                wm, B_wm = mod_alloc(ls)
                for g in range(12):
                    mod_group(i, g, wm, B_wm)
                mod_finish(i)

        def mod_finish(i):
            modT, A_m, A_f, G_m, G_f, lamv, B_mod, B_lam = mods[i % 2]
            if True:
                psv = ps[5][:, 0:96].rearrange("p (j s) -> p j s", s=2)
                bm = vecs[:, _voff(i, "bmod"):_voff(i, "bmod") + 48]
                for s in range(2):
                    S.op("dve", lambda e: e.tensor_tensor(out=modT[:, :, s], in0=psv[:, :, s], in1=bm, op=ALU.add),
                         reads=[B_ps[5], B_vecs], writes=[B_mod])
                for s in range(2):
                    for (dst, sc_off, gname) in ((A_m, 8, "gpm"), (A_f, 32, "gpf")):
                        gv = vecs[:, _voff(i, gname):_voff(i, gname) + 8]
                        S.op("dve", lambda e: e.scalar_tensor_tensor(out=dst[:, :, s], in0=modT[:, sc_off:sc_off + 8, s], scalar=1.0,
                                                                     in1=gv, op0=ALU.add, op1=ALU.mult),
                             reads=[B_mod, B_vecs], writes=[B_mod])
                    for (dst, gt_off, gname) in ((G_m, 16, "gqm"), (G_f, 40, "gqf")):
                        gv = vecs[:, _voff(i, gname):_voff(i, gname) + 8]
                        S.op("dve", lambda e: e.tensor_tensor(out=dst[:, :, s], in0=modT[:, gt_off:gt_off + 8, s], in1=gv, op=ALU.mult),
                             reads=[B_mod, B_vecs], writes=[B_mod])
                lo = _voff(i, "lam")
                S.op("dve", lambda e: e.tensor_tensor(out=lamv[:, 2:3], in0=vecs[:, lo:lo + 1], in1=vecs[:, lo + 1:lo + 2], op=ALU.mult),
                     reads=[B_vecs], writes=[B_lam])
                S.op("dve", lambda e: e.tensor_tensor(out=lamv[:, 3:4], in0=vecs[:, lo + 2:lo + 3], in1=vecs[:, lo + 3:lo + 4], op=ALU.mult),
                     reads=[B_vecs, B_lam], writes=[B_lam])
                S.op("pe", lambda e: e.matmul(ps[6][:, 0:2], ones_f[:], lamv[:, 2:4], start=True, stop=True),
                     sig=True, reads=[B_lam, B_const], writes=[B_ps[6]])
                S.op("act", lambda e: e.activation(out=lamv[:, 2:4], in_=ps[6][:, 0:2], func=AF.Exp), reads=[B_ps[6]], writes=[B_lam])
                lam_init = 0.8 - 0.6 * math.exp(-0.3 * i)
                S.op("dve", lambda e: e.scalar_tensor_tensor(out=lamv[:, 0:1], in0=lamv[:, 3:4], scalar=-lam_init, in1=lamv[:, 2:3],
                                                             op0=ALU.add, op1=ALU.subtract), reads=[B_lam], writes=[B_lam])
                S.op("dve", lambda e: e.tensor_scalar(out=lamv[:, 1:2], in0=vcol(i, "gsub"), scalar1=(1.0 - lam_init), scalar2=None,
                                                      op0=ALU.mult), reads=[B_vecs, B_lam], writes=[B_lam])

        def rstd_from_ss(ss_ps_ap, B_ss, n_feat, dst, B_dst, tmp, B_tmp):
            S.op("act", lambda e: e.activation(out=tmp, in_=ss_ps_ap, func=AF.Sqrt, scale=1.0 / n_feat, bias=epsc[:, 0:1]),
                 reads=[B_ss, B_const], writes=[B_tmp])
            S.op("dve", lambda e: e.reciprocal(out=dst, in_=tmp), reads=[B_tmp], writes=[B_dst])

        epsc = sb(st, "epsc", [128, 1], F32)
        S.op("dve", lambda e: e.memset(epsc[:], EPS), writes=[B_const])

        def norm_load(ls_bufs, src_view, B_src, c0s, N):
            S.dma("sp", ls_bufs[0][:, :, 0:N], src_view[:, :, c0s:c0s + N], reads=[B_src], writes=[ls_bufs[1]])

        def norm_mod_chunk(ls_bufs, src_view, B_src, c0s, N, Acol, Bcol_off, s, out_bf, B_out, out_f32=None, do_load=True):
            xc, B_xc, sq, B_sq, rs, B_rs, tmp, B_tmp, t2, B_t2 = ls_bufs
            if do_load:
                S.dma("sp", xc[:, :, 0:N], src_view[:, :, c0s:c0s + N], reads=[B_src], writes=[B_xc])
            for k in range(8):
                S.op("act", lambda e: e.activation(out=sq[:, k, 0:N], in_=xc[:, k, 0:N], func=AF.Square), reads=[B_xc], writes=[B_sq])
            for k in range(8):
                S.op("pe", lambda e: e.matmul(ps[7][:, 0:N], ones_bf[:], sq[:, k, 0:N], start=(k == 0), stop=(k == 7)),
                     sig=(k == 7), reads=[B_sq, B_const], writes=[B_ps[7]])
            rstd_from_ss(ps[7][:, 0:N], B_ps[7], D, rs[:, 0:N], B_rs, tmp[:, 0:N], B_tmp)
            nt2 = t2.shape[1]
            for k in range(8):
                kk = k % nt2
                S.op("dve", lambda e: e.scalar_tensor_tensor(out=t2[:, kk, 0:N], in0=xc[:, k, 0:N], scalar=Acol[:, k, s:s + 1], in1=rs[:, 0:N],
                                                             op0=ALU.mult, op1=ALU.mult), reads=[B_xc, B_rs, B_mod], writes=[B_t2[kk]])
                dst = out_bf(k)
                S.op("act", lambda e: e.activation(out=dst, in_=t2[:, kk, 0:N], func=AF.Identity,
                                                   bias=modT[:, Bcol_off + k, s:s + 1], scale=1.0),
                     reads=[B_t2[kk], B_mod], writes=[B_out])

        def alloc_norm_bufs(ls, tag, nt2=2):
            xc = sb(ls, "xc" + tag, [128, 8, 512], F32)
            sq = sb(ls, "sq" + tag, [128, 8, 512], BF16)
            rs = sb(ls, "rs" + tag, [128, 512], F32)
            tmp = sb(ls, "tmp" + tag, [128, 512], F32)
            t2 = sb(ls, "t2" + tag, [128, nt2, 512], F32)
            return (xc, S.buf("xc" + tag, True), sq, S.buf("sq" + tag), rs, S.buf("rs" + tag), tmp, S.buf("tmp" + tag),
                    t2, [S.buf(f"t2{tag}{q}") for q in range(nt2)])

        x_src, B_xs = xT_in, B_xin
        y_src, B_ys = ctxT_in, B_cin
        for i in range(n_layers):
            last = i == DEPTH - 1
            lam_init = 0.8 - 0.6 * math.exp(-0.3 * i)
            x_mid, B_xm = xA, B_xA
            y_mid, B_ym = yA, B_yA
            x_dst, B_xd = (outT, B_out) if last else (xB, B_xB)
            y_dst, B_yd = yB, B_yB
            modT, A_m, A_f, G_m, G_f, lamv, B_mod, B_lam = mods[i % 2]
            if i == 0:
                compute_mod(0)
            S.stop(f"{i}:mod")

            with ExitStack() as lay:
                KT = sb(lay, "KT", [128, 4, NTOK], BF16)
                Vt = sb(lay, "Vt", [128, 34, 512], BF16)
                B_KT, B_Vt = S.buf("KT"), S.buf("Vt")
                with ExitStack() as pa:
                    HT = sb(pa, "HT", [128, 8, NTOK], BF16)
                    B_HT = S.buf("HT")
                    with ExitStack() as pa1:
                        nb0 = alloc_norm_bufs(pa1, "a0")
                        xc_b = sb(pa1, "xca1", [128, 8, 512], F32)
                        nb = [nb0, (xc_b, S.buf("xca1", True)) + tuple(nb0[2:])]

                        def a1_load(cj):
                            c0j, Nj, iscj = CHUNKS[cj]
                            norm_load(nb[cj % 2], xview(y_src if iscj else x_src), B_ys if iscj else B_xs, 0 if iscj else c0j, Nj)
                        a1_load(0)
                        for ci, (c0, N, isc) in enumerate(CHUNKS):
                            if ci + 1 < len(CHUNKS):
                                a1_load(ci + 1)
                            src = xview(y_src if isc else x_src)
                            norm_mod_chunk(nb[ci % 2], src, B_ys if isc else B_xs, 0 if isc else c0, N, A_m, 0, 1 if isc else 0,
                                           lambda k: HT[:, k, c0:c0 + N], B_HT, do_load=False)
                    S.barrier()
                    S.stop(f"{i}:A1")
                    if dbg and i == 0:
                        dbg_HT = nc.dram_tensor("dbg_HT", [8, 128, NTOK], BF16, kind="ExternalOutput").ap()
                        B_dbg = S.buf("dbg", True)
                        for k in range(8):
                            S.dma("sp", dbg_HT[k], HT[:, k, :], reads=[B_HT], writes=[B_dbg])
                    with ExitStack() as pa2:
                        wt = [sb(pa2, f"wt{b}", [128, 8, 512], BF16) for b in range(2)]
                        B_wt = [S.buf(f"wt{b}", True) for b in range(2)]
                        NST = 12
                        stg = [sb(pa2, f"stg{b}", [128, 512], BF16) for b in range(NST)]
                        B_stg = [S.buf(f"stg{b}", True) for b in range(NST)]
                        qb = [sb(pa2, f"qb{b}", [128, 512], BF16) for b in range(2)]
                        B_qb = [S.buf(f"qb{b}") for b in range(2)]
                        r1 = [sb(pa2, f"r1{b}", [128, 512], F32) for b in range(2)]
                        B_r1 = [S.buf(f"r1{b}") for b in range(2)]
                        r2 = [sb(pa2, f"r2{b}", [128, 512], F32) for b in range(2)]
                        B_r2 = [S.buf(f"r2{b}") for b in range(2)]
                        wv = W["w_in"][i].rearrange("(k p) n -> p k n", p=128)
                        cosb = sb(pa2, "cosb", [128, T], BF16)
                        sinb = sb(pa2, "sinb", [128, T], BF16)
                        B_tab = S.buf("ropetab", True)
                        for hh in range(4):
                            S.dma("pool", cosb[:, hh * 1024:(hh + 1) * 1024], cos_in[:, hh * 1024:(hh + 1) * 1024], writes=[B_tab])
                            S.dma("pool", sinb[:, hh * 1024:(hh + 1) * 1024], sin_in[:, hh * 1024:(hh + 1) * 1024], writes=[B_tab])
                        cnt = 0
                        import os
                        glist = [int(v) for v in os.environ.get("KDBG_G", "0,1,2,3,4,5,6,7,8,9,10,11,12").split(",")]
                        for g in glist:
                            b = g % 2
                            S.dma("pool", wt[b][:], wv[:, :, g * 512:(g + 1) * 512], writes=[B_wt[b]])
                            if g == 2:
                                for tt in range(34):
                                    pb_ = 4 + (tt % 2)
                                    for k in range(8):
                                        S.op("pe", lambda e: e.matmul(ps[pb_][:], HT[:, k, tt * 128:(tt + 1) * 128], wt[b][:, k, :],
                                                                      start=(k == 0), stop=(k == 7)),
                                             sig=(k == 7), reads=[B_HT, B_wt[b]], writes=[B_ps[pb_]])
                                    if tt % 2 == 0:
                                        S.op("act", lambda e: e.activation(out=Vt[:, tt, :], in_=ps[pb_][:], func=AF.Copy),
                                             reads=[B_ps[pb_]], writes=[B_Vt])
                                    else:
                                        S.op("dve", lambda e: e.tensor_copy(out=Vt[:, tt, :], in_=ps[pb_][:]),
                                             reads=[B_ps[pb_]], writes=[B_Vt])
                                continue
                            for jj in range(4):
                                j = g * 4 + jj
                                for ci, (c0, N, isc) in enumerate(CHUNKS):
                                    if isc and last and g != 1:
                                        continue
                                    pb_ = cnt % 4
                                    cnt += 1
                                    for k in range(8):
                                        S.op("pe", lambda e: e.matmul(ps[pb_][:, 0:N], wt[b][:, k, jj * 128:(jj + 1) * 128], HT[:, k, c0:c0 + N],
                                                                      start=(k == 0), stop=(k == 7)),
                                             sig=(k == 7), reads=[B_HT, B_wt[b]], writes=[B_ps[pb_]])
                                    if g <= 1:
                                        if g == 0:
                                            sidx = cnt % NST
                                            dst, B_dst = stg[sidx][:, 0:N], B_stg[sidx]
                                        else:
                                            dst, B_dst = KT[:, jj, c0:c0 + N], B_KT
                                        kv = os.environ.get("KDBG_V", "4")
                                        if isc or kv == "1":
                                            S.op("act", lambda e: e.activation(out=dst, in_=ps[pb_][:, 0:N], func=AF.Copy),
                                                 reads=[B_ps[pb_]], writes=[B_dst])
                                        else:
                                            rb = cnt % 2
                                            S.op("act", lambda e: e.activation(out=qb[rb][:, 0:N], in_=ps[pb_][:, 0:N], func=AF.Copy),
                                                 reads=[B_ps[pb_]], writes=[B_qb[rb]])
                                            pp_ = 4 + rb
                                            if kv != "2":
                                                S.op("pe", lambda e: e.matmul(ps[pp_][:, 0:N], pm[:], qb[rb][:, 0:N], start=True, stop=True),
                                                     sig=True, reads=[B_qb[rb], B_const], writes=[B_ps[pp_]])
                                            else:
                                                pp_ = pb_
                                            if kv not in ("3", "4"):
                                                S.op("dve", lambda e: e.tensor_tensor(out=r1[rb][:, 0:N], in0=ps[pb_][:, 0:N], in1=cosb[:, c0:c0 + N], op=ALU.mult),
                                                     reads=[B_ps[pb_], B_tab], writes=[B_r1[rb]])
                                                S.op("dve", lambda e: e.tensor_tensor(out=r2[rb][:, 0:N], in0=ps[pp_][:, 0:N], in1=sinb[:, c0:c0 + N], op=ALU.mult),
                                                     reads=[B_ps[pp_], B_tab], writes=[B_r2[rb]])
                                            else:
                                                S.op("act", lambda e: e.activation(out=r1[rb][:, 0:N], in_=ps[pb_][:, 0:N], func=AF.Copy),
                                                     reads=[B_ps[pb_], B_tab], writes=[B_r1[rb]])
                                                S.op("act", lambda e: e.activation(out=r2[rb][:, 0:N], in_=ps[pp_][:, 0:N], func=AF.Copy),
                                                     reads=[B_ps[pp_], B_tab], writes=[B_r2[rb]])
                                            if kv == "4":
                                                S.op("dve", lambda e: e.tensor_tensor(out=r1[rb][:, 0:N], in0=r1[rb][:, 0:N], in1=cosb[:, c0:c0 + N], op=ALU.mult),
                                                     reads=[B_r1[rb], B_tab], writes=[B_r1[rb]])
                                                S.op("dve", lambda e: e.tensor_tensor(out=r2[rb][:, 0:N], in0=r2[rb][:, 0:N], in1=sinb[:, c0:c0 + N], op=ALU.mult),
                                                     reads=[B_r2[rb], B_tab], writes=[B_r2[rb]])
                                            S.op(os.environ.get("KDBG_ROPE", "pool"), lambda e: e.tensor_tensor(out=dst, in0=r1[rb][:, 0:N], in1=r2[rb][:, 0:N], op=ALU.add),
                                                 reads=[B_r1[rb], B_r2[rb]], writes=[B_dst])
                                        if g == 0:
                                            S.dma("sp", QT[jj, :, c0:c0 + N], dst, reads=[B_dst], writes=[B_QT], sem_buf=B_dst)
                                    else:
                                        sidx = cnt % NST
                                        zc0 = (ZC if isc else ZL + c0)
                                        if g >= 7:
                                            S.op("act", lambda e: e.activation(out=stg[sidx][:, 0:N], in_=ps[pb_][:, 0:N], func=AF.Sigmoid),
                                                 reads=[B_ps[pb_]], writes=[B_stg[sidx]])
                                        elif cnt % 2 == 0:
                                            S.op("act", lambda e: e.activation(out=stg[sidx][:, 0:N], in_=ps[pb_][:, 0:N], func=AF.Copy),
                                                 reads=[B_ps[pb_]], writes=[B_stg[sidx]])
                                        else:
                                            S.op("dve", lambda e: e.tensor_copy(out=stg[sidx][:, 0:N], in_=ps[pb_][:, 0:N]),
                                                 reads=[B_ps[pb_]], writes=[B_stg[sidx]])
                                        S.dma("sp", ZT[j - 12, :, zc0:zc0 + N], stg[sidx][:, 0:N], reads=[B_stg[sidx]], writes=[B_ZT], sem_buf=B_stg[sidx])
                    S.barrier()
                    S.stop(f"{i}:A2")
                with ExitStack() as pb:
                    qz = [[sb(pb, f"qz{m}{b}", [128, 4, 512], BF16) for b in range(2)] for m in range(2)]
                    B_qz = [S.buf(f"qz{b}", True) for b in range(2)]
                    for b_ in range(2):
                        S.op("pool", lambda e: e.memset(qz[0][b_][64:128, :, :], 0.0), writes=[B_qz[b_]])
                        S.op("pool", lambda e: e.memset(qz[1][b_][0:64, :, :], 0.0), writes=[B_qz[b_]])
                    Pt = [[sb(pb, f"P{m}{b}", [128, 512], BF16) for b in range(3)] for m in range(2)]
                    B_P = [[S.buf(f"P{m}{b}") for b in range(3)] for m in range(2)]
                    pacc = [sb(pb, f"pacc{m}", [128, 512], F32) for m in range(2)]
                    B_pacc = [S.buf(f"pacc{m}") for m in range(2)]
                    first_ci = 0

                    def load_q(cj):
                        c0j, Nj, _ = CHUNKS[cj]
                        bj = cj % 2
                        S.dma("sp", qz[0][bj][0:64, :, 0:Nj], QT[:, 0:64, c0j:c0j + Nj].rearrange("h p n -> p h n"), reads=[B_QT], writes=[B_qz[bj]])
                        S.dma("sp", qz[1][bj][64:128, :, 0:Nj], QT[:, 64:128, c0j:c0j + Nj].rearrange("h p n -> p h n"), reads=[B_QT], writes=[B_qz[bj]])
                    rl = [sb(pb, f"rl{m}", [128, 512], F32) for m in range(2)]
                    B_rl = [S.buf(f"rl{m}") for m in range(2)]
                    a1 = sb(pb, "a1", [128, 512], F32); B_a1 = S.buf("a1")
                    a2 = sb(pb, "a2", [128, 512], F32); B_a2 = S.buf("a2")
                    aa = sb(pb, "aa", [128, 512], F32); B_aa = S.buf("aa")
                    asq = sb(pb, "asq", [128, 512], BF16); B_asq = S.buf("asq")
                    tmpb = sb(pb, "tmpb", [128, 512], F32); B_tmpb = S.buf("tmpb")
                    rsb = sb(pb, "rsb", [128, 512], F32); B_rsb = S.buf("rsb")
                    attn_o = sb(pb, "attn_o", [128, 4, 512], BF16); B_ao = S.buf("attn_o")
                    for ci, (c0, N, isc) in enumerate(CHUNKS):
                        if isc and last:
                            continue
                        s = 1 if isc else 0
                        qb_ = ci % 2
                        if ci == first_ci:
                            load_q(ci)
                        nxt = [c_ for c_ in range(ci + 1, len(CHUNKS)) if not (CHUNKS[c_][2] and last)]
                        if nxt:
                            load_q(nxt[0])
                        kts = [32, 33] if isc else list(range(34))
                        nk = len(kts)
                        SB = [[0, 1], [2, 3]]
                        NSB = 2
                        LB = [6, 7]

                        def qk(h, idx):
                            kt = kts[idx]
                            for m in range(2):
                                bank = SB[m][idx % NSB]
                                S.op("pe", lambda e: e.matmul(ps[bank][:, 0:N], KT[:, h, kt * 128:(kt + 1) * 128], qz[m][qb_][:, h, 0:N], start=True, stop=True),
                                     sig=True, reads=[B_KT, B_qz[qb_]], writes=[B_ps[bank]])

                        def post(h):
                            for m in range(2):
                                S.op("dve", lambda e: e.reciprocal(out=rl[m][:, 0:N], in_=ps[LB[m]][:, 0:N]), reads=[B_ps[LB[m]]], writes=[B_rl[m]])
                            S.op("dve", lambda e: e.tensor_tensor(out=a1[:, 0:N], in0=ps[4][:, 0:N], in1=rl[0][:, 0:N], op=ALU.mult),
                                 reads=[B_ps[4], B_rl[0]], writes=[B_a1])
                            S.op("dve", lambda e: e.tensor_tensor(out=a2[:, 0:N], in0=ps[5][:, 0:N], in1=rl[1][:, 0:N], op=ALU.mult),
                                 reads=[B_ps[5], B_rl[1]], writes=[B_a2])
                            S.op("dve", lambda e: e.scalar_tensor_tensor(out=aa[:, 0:N], in0=a2[:, 0:N], scalar=lamv[:, 0:1], in1=a1[:, 0:N],
                                                                         op0=ALU.mult, op1=ALU.add), reads=[B_a1, B_a2, B_lam], writes=[B_aa])
                            S.op("act", lambda e: e.activation(out=asq[:, 0:N], in_=aa[:, 0:N], func=AF.Square), reads=[B_aa], writes=[B_asq])
                            S.op("pe", lambda e: e.matmul(ps[LB[1]][:, 0:N], ones_bf[:], asq[:, 0:N], start=True, stop=True),
                                 sig=True, reads=[B_asq, B_const], writes=[B_ps[LB[1]]])
                            rstd_from_ss(ps[LB[1]][:, 0:N], B_ps[LB[1]], 128, rsb[:, 0:N], B_rsb, tmpb[:, 0:N], B_tmpb)
                            S.op("dve", lambda e: e.scalar_tensor_tensor(out=attn_o[:, h, 0:N], in0=aa[:, 0:N], scalar=lamv[:, 1:2], in1=rsb[:, 0:N],
                                                                         op0=ALU.mult, op1=ALU.mult), reads=[B_aa, B_rsb, B_lam], writes=[B_ao])

                        qk(0, 0)
                        if nk > 1:
                            qk(0, 1)
                        for h in range(4):
                            for idx in range(nk):
                                kt = kts[idx]
                                if idx + 1 < nk and idx >= 1:
                                    qk(h, idx + 1)
                                for m in range(2):
                                    bank = SB[m][idx % NSB]
                                    S.op("act", lambda e: e.activation(out=Pt[m][idx % 3][:, 0:N], in_=ps[bank][:, 0:N], func=AF.Exp, scale=0.125),
                                         reads=[B_ps[bank]], writes=[B_P[m][idx % 3]])
                                for m in range(2):
                                    S.op("pe", lambda e: e.matmul(ps[4 + m][:, 0:N], Vt[:, kt, h * 128:(h + 1) * 128], Pt[m][idx % 3][:, 0:N],
                                                                  start=(idx == 0), stop=(idx == nk - 1)),
                                         sig=(idx == nk - 1), reads=[B_Vt, B_P[m][idx % 3]], writes=[B_ps[4 + m]])
                                    if m == 0:
                                        S.op("pe", lambda e: e.matmul(ps[LB[0]][:, 0:N], ones_bf[:], Pt[0][idx % 3][:, 0:N],
                                                                      start=(idx == 0), stop=(idx == nk - 1)),
                                             sig=(idx == nk - 1), reads=[B_const, B_P[0][idx % 3]], writes=[B_ps[LB[0]]])
                                    else:
                                        q_ = idx % 2
                                        en_ = "dve" if q_ == 0 else "pool"
                                        if idx < 2:
                                            S.op(en_, lambda e: e.tensor_copy(out=pacc[q_][:, 0:N], in_=Pt[1][idx % 3][:, 0:N]), reads=[B_P[1][idx % 3]], writes=[B_pacc[q_]])
                                        else:
                                            S.op(en_, lambda e: e.tensor_tensor(out=pacc[q_][:, 0:N], in0=pacc[q_][:, 0:N], in1=Pt[1][idx % 3][:, 0:N], op=ALU.add),
                                                 reads=[B_P[1][idx % 3], B_pacc[q_]], writes=[B_pacc[q_]])
                            S.op("pe", lambda e: e.matmul(ps[LB[1]][:, 0:N], ones_f[:], pacc[0][:, 0:N], start=True, stop=False),
                                 sig=False, reads=[B_pacc[0], B_const], writes=[B_ps[LB[1]]])
                            S.op("pe", lambda e: e.matmul(ps[LB[1]][:, 0:N], ones_f[:], pacc[1][:, 0:N], start=False, stop=True),
                                 sig=True, reads=[B_pacc[1], B_const], writes=[B_ps[LB[1]]])
                            if h + 1 < 4:
                                qk(h + 1, 0)
                                if nk > 1:
                                    qk(h + 1, 1)
                            post(h)
                        S.dma("sp", AOT[:, :, c0:c0 + N].rearrange("h p n -> p h n"), attn_o[:, :, 0:N], reads=[B_ao], writes=[B_AOT])
                    S.barrier()
                    S.stop(f"{i}:B1")
            with ExitStack() as pb:
                wbr = sb(pb, "wbr", [128, 12, 1024], BF16)
                wout = sb(pb, "wout", [128, 8, 1024], BF16)
                poolw = sb(pb, "poolw", [128, 4, 128], BF16)
                B_wB = S.buf("wB", True)
                for r in range(3):
                    S.dma("pool", wbr[:, r * 4:(r + 1) * 4, :], W["w_branch"][i, r].rearrange("(k p) n -> p k n", p=128), writes=[B_wB])
                S.dma("pool", wout[:], W["w_out"][i].rearrange("(k p) n -> p k n", p=128), writes=[B_wB])
                S.dma("pool", poolw[:], W["pool_w"][i].rearrange("g c d -> c g d"), writes=[B_wB])
                attn_o = sb(pb, "attn_o", [128, 4, 512], BF16); B_ao = S.buf("attn_o2", True)
                tmpb = sb(pb, "tmpb", [128, 512], F32); B_tmpb = S.buf("tmpb")
                rsb = sb(pb, "rsb", [128, 512], F32); B_rsb = S.buf("rsb")
                conv_o = sb(pb, "conv_o", [128, 4, 512], BF16); B_co = S.buf("conv_o")
                pool_o = sb(pb, "pool_o", [128, 4, 512], BF16); B_po = S.buf("pool_o")
                cbt = sb(pb, "cbt", [128, 4, 512], BF16); B_cbt = S.buf("cbt", True)
                cct = sb(pb, "cct", [128, 4, 514], BF16); B_cct = S.buf("cct", True)
                cxt = sb(pb, "cxt", [128, 4, 514], BF16); B_cxt = S.buf("cxt", True)
                ut = sb(pb, "ut", [128, 4, 514], F32); B_ut = S.buf("ut")
                cv = sb(pb, "cv", [128, 4, 512], F32); B_cv = S.buf("cv")
                pint = sb(pb, "pint", [128, 4, 528], BF16); B_pin = S.buf("pint", True)
                w1 = sb(pb, "w1", [128, 4, 528], F32); B_w1 = [S.buf(f"w1{g}") for g in range(4)]
                w2 = sb(pb, "w2", [128, 4, 528], F32); B_w2 = [S.buf(f"w2{g}") for g in range(4)]
                ppb = sb(pb, "ppb", [128, 4, 512], BF16); B_pp = S.buf("ppb")
                invs = sb(pb, "invs", [128, 4, 512], F32); B_inv = S.buf("invs", True)
                mg2 = [[sb(pb, f"mg{r}{b}", [128, 512], F32) for r in range(3)] for b in range(2)]
                B_mg2 = [[S.buf(f"mg{r}{b}") for r in range(3)] for b in range(2)]
                B_mixj = [S.buf(f"mixT{j}") for j in range(8)]
                merged = sb(pb, "merged", [128, 8, 512], BF16); B_mer = S.buf("merged")
                mixT = sb(pb, "mixT", [128, 8, 512], F32); B_mix = S.buf("mixT")
                msq = sb(pb, "msq", [128, 8, 512], BF16); B_msq = S.buf("msq")
                xcb = sb(pb, "xcb", [128, 8, 512], F32); B_xcb = S.buf("xcb", True)

                conv_o2 = [conv_o, sb(pb, "conv_o_b", [128, 4, 512], BF16)]; B_co2 = [B_co, S.buf("conv_o_b")]
                ppb2 = [ppb, sb(pb, "ppb_b", [128, 4, 512], BF16)]; B_pp2 = [B_pp, S.buf("ppb_b")]
                gj = [sb(pb, f"gj{b}", [128, 3, 512], BF16) for b in range(2)]; B_gj = [S.buf(f"gj{b}", True) for b in range(2)]
                cw = _voff(i, "convw")
                pso = _voff(i, "pscale")
                b2_chunks = [ci for ci, c in enumerate(CHUNKS) if not (c[2] and last)]

                def load_ao(ci):
                    c0, N, isc = CHUNKS[ci]
                    S.dma("sp", attn_o[:, :, 0:N], AOT[:, :, c0:c0 + N].rearrange("h p n -> p h n"), reads=[B_AOT], writes=[B_ao])

                def stageX(ci):
                    c0, N, isc = CHUNKS[ci]
                    xb = ci % 2
                    zc0 = ZC if isc else ZL + c0
                    S.dma("sp", cbt[:, :, 0:N], ZT[0:4, :, zc0:zc0 + N].rearrange("j p n -> p j n"), reads=[B_ZT], writes=[B_cbt])
                    S.dma("sp", cct[:, :, 0:N + 2], ZT[4:8, :, zc0 - 1:zc0 + N + 1].rearrange("j p n -> p j n"), reads=[B_ZT], writes=[B_cct])
                    S.dma("sp", cxt[:, :, 0:N + 2], ZT[8:12, :, zc0 - 1:zc0 + N + 1].rearrange("j p n -> p j n"), reads=[B_ZT], writes=[B_cxt])
                    S.dma("sp", pint[:, :, 0:N + 16], ZT[12:16, :, zc0 - 8:zc0 + N + 8].rearrange("j p n -> p j n"), reads=[B_ZT], writes=[B_pin])
                    tbl = 2 if isc else (0 if ci == 0 else (1 if ci == 7 else None))
                    if tbl is not None:
                        S.dma("sp", invs[:], invt_in[:, tbl], writes=[B_inv])
                    S.op("pool", lambda e: e.tensor_tensor(out=ut[:, :, 0:N + 2], in0=cct[:, :, 0:N + 2], in1=cxt[:, :, 0:N + 2], op=ALU.mult),
                         reads=[B_cct, B_cxt], writes=[B_ut])

                    def padd(dst, B_d, a, b_, Bs):
                        S.op("pool", lambda e: e.tensor_tensor(out=dst, in0=a, in1=b_, op=ALU.add), reads=Bs, writes=[B_d])
                    M = N
                    padd(w1[:, 0, 8:M + 8], B_w1[0], pint[:, 0, 7:M + 7], pint[:, 0, 8:M + 8], [B_pin])
                    for g in range(1, 4):
                        padd(w1[:, g, 1:M + 16], B_w1[g], pint[:, g, 0:M + 15], pint[:, g, 1:M + 16], [B_pin])
                    padd(w2[:, 1, 8:M + 8], B_w2[1], w1[:, 1, 7:M + 7], w1[:, 1, 9:M + 9], [B_w1[1]])
                    for g in (2, 3):
                        padd(w2[:, g, 2:M + 15], B_w2[g], w1[:, g, 1:M + 14], w1[:, g, 3:M + 16], [B_w1[g]])
                    padd(w1[:, 2, 8:M + 8], B_w1[2], w2[:, 2, 6:M + 6], w2[:, 2, 10:M + 10], [B_w2[2]])
                    padd(w1[:, 3, 4:M + 13], B_w1[3], w2[:, 3, 2:M + 11], w2[:, 3, 6:M + 15], [B_w2[3]])
                    padd(w2[:, 3, 8:M + 8], B_w2[3], w1[:, 3, 4:M + 4], w1[:, 3, 12:M + 12], [B_w1[3]])
                    for ct in range(4):
                        S.op("dve", lambda e: e.tensor_scalar(out=cv[:, ct, 0:N], in0=ut[:, ct, 1:N + 1], scalar1=vecs[:, cw + 4 + ct:cw + 5 + ct],
                                                              scalar2=None, op0=ALU.mult), reads=[B_ut, B_vecs], writes=[B_cv])
                        S.op("dve", lambda e: e.scalar_tensor_tensor(out=cv[:, ct, 0:N], in0=ut[:, ct, 0:N], scalar=vecs[:, cw + ct:cw + ct + 1],
                                                                     in1=cv[:, ct, 0:N], op0=ALU.mult, op1=ALU.add),
                             reads=[B_ut, B_vecs, B_cv], writes=[B_cv])
                        S.op("dve", lambda e: e.scalar_tensor_tensor(out=cv[:, ct, 0:N], in0=ut[:, ct, 2:N + 2], scalar=vecs[:, cw + 8 + ct:cw + 9 + ct],
                                                                     in1=cv[:, ct, 0:N], op0=ALU.mult, op1=ALU.add),
                             reads=[B_ut, B_vecs, B_cv], writes=[B_cv])
                    S.op("pool", lambda e: e.tensor_tensor(out=conv_o2[xb][:, :, 0:N], in0=cv[:, :, 0:N], in1=cbt[:, :, 0:N], op=ALU.mult),
                         reads=[B_cv, B_cbt], writes=[B_co2[xb]])
                    fin = [(w1, B_w1[0]), (w2, B_w2[1]), (w1, B_w1[2]), (w2, B_w2[3])]
                    for g, wsz in enumerate((2, 4, 8, 16)):
                        src_t, B_s = fin[g]
                        if tbl is None:
                            S.op("dve", lambda e: e.scalar_tensor_tensor(out=ppb2[xb][:, g, 0:N], in0=src_t[:, g, 8:N + 8], scalar=1.0 / wsz,
                                                                         in1=pint[:, g, 8:N + 8], op0=ALU.mult, op1=ALU.subtract),
                                 reads=[B_s, B_pin], writes=[B_pp2[xb]])
                        else:
                            S.op("dve", lambda e: e.tensor_tensor(out=src_t[:, g, 8:N + 8], in0=src_t[:, g, 8:N + 8], in1=invs[:, g, 0:N], op=ALU.mult),
                                 reads=[B_s, B_inv], writes=[B_s])
                            S.op("dve", lambda e: e.tensor_tensor(out=ppb2[xb][:, g, 0:N], in0=src_t[:, g, 8:N + 8], in1=pint[:, g, 8:N + 8], op=ALU.subtract),
                                 reads=[B_s, B_pin], writes=[B_pp2[xb]])

                def load_g(ci, j):
                    c0, N, isc = CHUNKS[ci]
                    zc0 = ZC if isc else ZL + c0
                    for r in range(3):
                        S.dma("sp", gj[j % 2][:, r, 0:N], ZT[16 + r * 8 + j, :, zc0:zc0 + N], reads=[B_ZT], writes=[B_gj[j % 2]])

                def stageY(ci, nxt):
                    c0, N, isc = CHUNKS[ci]
                    s = 1 if isc else 0
                    xb = ci % 2
                    srcv = xview(y_src if isc else x_src)
                    cs = 0 if isc else c0
                    S.dma("sp", xcb[:, :, 0:N], srcv[:, :, cs:cs + N], reads=[B_ys if isc else B_xs], writes=[B_xcb])
                    load_g(ci, 0)
                    for g in range(4):
                        S.op("pe", lambda e: e.matmul(ps[0][:, 0:N], poolw[:, g, :], ppb2[xb][:, g, 0:N], start=True, stop=True), sig=True,
                             reads=[B_wB, B_pp2[xb]], writes=[B_ps[0]])
                        S.op("act", lambda e: e.activation(out=pool_o[:, g, 0:N], in_=ps[0][:, 0:N], func=AF.Identity, scale=vecs[:, pso + g:pso + g + 1]),
                             reads=[B_ps[0], B_vecs], writes=[B_po])
                    brs = [(attn_o, B_ao), (conv_o2[xb], B_co2[xb]), (pool_o, B_po)]
                    bset = [[1, 2, 3], [4, 5, 6]]

                    def branches(j):
                        for r in range(3):
                            bk = bset[j % 2][r]
                            for k in range(4):
                                S.op("pe", lambda e: e.matmul(ps[bk][:, 0:N], wbr[:, r * 4 + k, j * 128:(j + 1) * 128], brs[r][0][:, k, 0:N],
                                                              start=(k == 0), stop=(k == 3)), sig=(k == 3),
                                     reads=[B_wB, brs[r][1]], writes=[B_ps[bk]])
                    branches(0)
                    for j in range(8):
                        if j + 1 < 8:
                            load_g(ci, j + 1)
                        mg, B_mg = mg2[j % 2], B_mg2[j % 2]
                        mgb, B_mgb = mg, B_mg
                        for r in range(3):
                            bk = bset[j % 2][r]
                            S.op("act", lambda e: e.activation(out=mg[r][:, 0:N], in_=ps[bk][:, 0:N], func=AF.Copy),
                                 reads=[B_ps[bk]], writes=[B_mg[r]])
                            S.op("dve", lambda e: e.tensor_tensor(out=mgb[r][:, 0:N], in0=mg[r][:, 0:N], in1=gj[j % 2][:, r, 0:N], op=ALU.mult),
                                 reads=[B_mg[r], B_gj[j % 2]], writes=[B_mgb[r]])
                        if j + 1 < 8:
                            branches(j + 1)
                        S.op("pool", lambda e: e.tensor_tensor(out=mgb[0][:, 0:N], in0=mgb[0][:, 0:N], in1=mgb[1][:, 0:N], op=ALU.add),
                             reads=[B_mgb[0], B_mgb[1]], writes=[B_mgb[0]])
                        S.op("pool", lambda e: e.tensor_tensor(out=merged[:, j, 0:N], in0=mgb[0][:, 0:N], in1=mgb[2][:, 0:N], op=ALU.add),
                             reads=[B_mgb[0], B_mgb[2]], writes=[B_mer])
                    if nxt is not None:
                        load_ao(nxt)
                    for j in range(8):
                        bank = 4 + (j % 2)
                        for k in range(8):
                            S.op("pe", lambda e: e.matmul(ps[bank][:, 0:N], wout[:, k, j * 128:(j + 1) * 128], merged[:, k, 0:N],
                                                          start=(k == 0), stop=(k == 7)), sig=(k == 7),
                                 reads=[B_wB, B_mer], writes=[B_ps[bank]])
                        S.op("act", lambda e: e.activation(out=mixT[:, j, 0:N], in_=ps[bank][:, 0:N], func=AF.Copy), reads=[B_ps[bank]], writes=[B_mixj[j]])
                        S.op("act", lambda e: e.activation(out=msq[:, j, 0:N], in_=ps[bank][:, 0:N], func=AF.Square), reads=[B_ps[bank]], writes=[B_msq])
                    for j in range(8):
                        S.op("pe", lambda e: e.matmul(ps[6][:, 0:N], ones_bf[:], msq[:, j, 0:N], start=(j == 0), stop=(j == 7)), sig=(j == 7),
                             reads=[B_msq, B_const], writes=[B_ps[6]])
                    rstd_from_ss(ps[6][:, 0:N], B_ps[6], D, rsb[:, 0:N], B_rsb, tmpb[:, 0:N], B_tmpb)
                    for j in range(8):
                        S.op("dve", lambda e: e.scalar_tensor_tensor(out=mixT[:, j, 0:N], in0=mixT[:, j, 0:N], scalar=G_m[:, j, s:s + 1], in1=rsb[:, 0:N],
                                                                     op0=ALU.mult, op1=ALU.mult), reads=[B_mixj[j], B_rsb, B_mod], writes=[B_mixj[j]])
                        S.op("pool", lambda e: e.tensor_tensor(out=mixT[:, j, 0:N], in0=mixT[:, j, 0:N], in1=xcb[:, j, 0:N], op=ALU.add),
                             reads=[B_mixj[j], B_xcb], writes=[B_mixj[j]])
                    dstv = xview(y_mid if isc else x_mid)
                    S.dma("sp", dstv[:, :, cs:cs + N], mixT[:, :, 0:N], reads=B_mixj, writes=[B_ym if isc else B_xm])

                load_ao(b2_chunks[0])
                stageX(b2_chunks[0])
                for n_, ci in enumerate(b2_chunks):
                    nxt = b2_chunks[n_ + 1] if n_ + 1 < len(b2_chunks) else None
                    if nxt is not None:
                        stageX(nxt)
                    stageY(ci, nxt)
                S.barrier()
                S.stop(f"{i}:B2")
            n_exp = 1 if i % 2 == 0 else NE
            moe = n_exp > 1
            ffn_chunks = [c for c in CHUNKS if not (c[2] and last)]
            with ExitStack() as pc1:
                nbf = alloc_norm_bufs(pc1, "c", 8)
                h2s = sb(pc1, "h2s", [128, 8, 512], BF16); B_h2s = S.buf("h2s")
                if moe:
                    ro = 16 + DEPTH * VEC_LAYER
                    lg = sb(pc1, "lg", [128, 8], F32); B_lg = S.buf("lg")
                    l2 = sb(pc1, "l2", [128, 8], F32); B_l2 = S.buf("l2")
                    mk1 = sb(pc1, "mk1", [128, 8], F32); B_mk1 = S.buf("mk1")
                    mk2 = sb(pc1, "mk2", [128, 8], F32); B_mk2 = S.buf("mk2")
                    st1 = sb(pc1, "st1", [128, 8], F32); B_st = S.buf("st1")
                    comb = sb(pc1, "comb", [128, 8], F32); B_comb = S.buf("comb")
                    combT = sb(pc1, "combT", [8, 512], F32); B_combT = S.buf("combT")
                    h2f = sb(pc1, "h2f", [128, 8, 512], F32); B_h2f = S.buf("h2f")
                    if SPARSE_MOE:
                        sel = sb(pc1, "sel", [128, 8], F32); B_sel = S.buf("sel")
                        selsum = sb(pc1, "selsum", [128, 8], F32); B_selsum = S.buf("selsum")
                        tmp8 = sb(pc1, "tmp8", [128, 2, 8], F32); B_tmp8 = S.buf("tmp8")
                        h2rows = sb(pc1, "h2rows", [128, 32, 1024], BF16); B_h2rows = S.buf("h2rows")
                        rt = sb(pc1, "rt", [128, 8, 32], F32); B_rt = S.buf("rt")
                        first_tile = [True]
                for ci, (c0, N, isc) in enumerate(ffn_chunks):
                    src = xview(y_mid if isc else x_mid)
                    norm_mod_chunk(nbf, src, B_ym if isc else B_xm, 0 if isc else c0, N, A_f, 24, 1 if isc else 0,
                                   lambda k: h2s[:, k, 0:N], B_h2s)
                    S.dma("sp", H2T[:, :, c0:c0 + N].rearrange("k p n -> p k n"), h2s[:, :, 0:N], reads=[B_h2s], writes=[B_H2T])
                    if moe:
                        t2 = nbf[8]; B_t2 = nbf[9]
                        for k in range(8):
                            S.op("dve", lambda e: e.tensor_scalar(out=h2f[:, k, 0:N], in0=t2[:, k, 0:N], scalar1=modT[:, 24 + k, 0:1], scalar2=None, op0=ALU.add),
                                 reads=[B_t2[k], B_mod], writes=[B_h2f])
                        for tt in range(N // 128):
                            for k in range(8):
                                S.op("pe", lambda e: e.matmul(ps[0][:, 0:8], h2f[:, k, tt * 128:(tt + 1) * 128], vecs[:, ro + k * 8:ro + k * 8 + 8],
                                                              start=(k == 0), stop=(k == 7)), sig=(k == 7), reads=[B_h2f, B_vecs], writes=[B_ps[0]])
                            S.op("dve", lambda e: e.tensor_copy(out=lg[:], in_=ps[0][:, 0:8]), reads=[B_ps[0]], writes=[B_lg])
                            S.op("dve", lambda e: e.reduce_max(out=st1[:, 0:1], in_=lg[:], axis=mybir.AxisListType.X), reads=[B_lg], writes=[B_st])
                            S.op("dve", lambda e: e.tensor_scalar(out=mk1[:], in0=lg[:], scalar1=st1[:, 0:1], scalar2=None, op0=ALU.is_equal),
                                 reads=[B_lg, B_st], writes=[B_mk1])
                            S.op("dve", lambda e: e.scalar_tensor_tensor(out=l2[:], in0=mk1[:], scalar=-1e30, in1=lg[:], op0=ALU.mult, op1=ALU.add),
                                 reads=[B_mk1, B_lg], writes=[B_l2])
                            S.op("dve", lambda e: e.reduce_max(out=st1[:, 1:2], in_=l2[:], axis=mybir.AxisListType.X), reads=[B_l2, B_st], writes=[B_st])
                            S.op("dve", lambda e: e.tensor_scalar(out=mk2[:], in0=l2[:], scalar1=st1[:, 1:2], scalar2=None, op0=ALU.is_equal),
                                 reads=[B_l2, B_st], writes=[B_mk2])
                            S.op("dve", lambda e: e.tensor_tensor(out=st1[:, 2:3], in0=st1[:, 1:2], in1=st1[:, 0:1], op=ALU.subtract), reads=[B_st], writes=[B_st])
                            S.op("act", lambda e: e.activation(out=st1[:, 3:4], in_=st1[:, 2:3], func=AF.Exp), reads=[B_st], writes=[B_st])
                            S.op("dve", lambda e: e.tensor_scalar(out=st1[:, 4:5], in0=st1[:, 3:4], scalar1=1.0, scalar2=None, op0=ALU.add), reads=[B_st], writes=[B_st])
                            S.op("dve", lambda e: e.reciprocal(out=st1[:, 5:6], in_=st1[:, 4:5]), reads=[B_st], writes=[B_st])
                            S.op("dve", lambda e: e.tensor_tensor(out=st1[:, 6:7], in0=st1[:, 3:4], in1=st1[:, 5:6], op=ALU.mult), reads=[B_st], writes=[B_st])
                            if not SPARSE_MOE:
                                S.op("dve", lambda e: e.tensor_scalar(out=comb[:], in0=mk1[:], scalar1=st1[:, 5:6], scalar2=None, op0=ALU.mult),
                                     reads=[B_mk1, B_st], writes=[B_comb])
                                S.op("dve", lambda e: e.scalar_tensor_tensor(out=comb[:], in0=mk2[:], scalar=st1[:, 6:7], in1=comb[:], op0=ALU.mult, op1=ALU.add),
                                     reads=[B_mk2, B_st, B_comb], writes=[B_comb])
                                S.op("pe", lambda e: e.matmul(ps[1][0:8, 0:128], comb[:], ident[:], start=True, stop=True),
                                     sig=True, reads=[B_comb, B_const], writes=[B_ps[1]])
                                S.op("act", lambda e: e.activation(out=combT[:, tt * 128:(tt + 1) * 128], in_=ps[1][0:8, 0:128], func=AF.Copy),
                                     reads=[B_ps[1]], writes=[B_combT])
                                continue
                            T_ = c0 // 128 + tt
                            S.op("dve", lambda e: e.tensor_copy(out=mk1s[:, T_, :], in_=mk1[:]), reads=[B_mk1], writes=[B_mks])
                            S.op("dve", lambda e: e.tensor_copy(out=mk2s[:, T_, :], in_=mk2[:]), reads=[B_mk2], writes=[B_mks])
                            S.op("dve", lambda e: e.tensor_tensor(out=sel[:], in0=mk1[:], in1=mk2[:], op=ALU.add), reads=[B_mk1, B_mk2], writes=[B_sel])
                            ft = first_tile[0]
                            S.op("pe", lambda e: e.matmul(ps[2][:, 0:8], triu[:], sel[:], start=True, stop=ft), sig=ft, reads=[B_sel, B_const], writes=[B_ps[2]])
                            if not ft:
                                S.op("pe", lambda e: e.matmul(ps[2][:, 0:8], ones_f[:], selsum[:], start=False, stop=True),
                                     sig=True, reads=[B_selsum, B_const], writes=[B_ps[2]])
                            S.op("dve", lambda e: e.tensor_tensor(out=tmp8[:, 0, :], in0=ps[2][:, 0:8], in1=mk1[:], op=ALU.mult), reads=[B_ps[2], B_mk1], writes=[B_tmp8])
                            S.op("dve", lambda e: e.tensor_tensor(out=tmp8[:, 1, :], in0=ps[2][:, 0:8], in1=mk2[:], op=ALU.mult), reads=[B_ps[2], B_mk2], writes=[B_tmp8])
                            S.op("dve", lambda e: e.reduce_sum(out=rtab[:, 0, T_:T_ + 1], in_=tmp8[:, 0, :], axis=mybir.AxisListType.X), reads=[B_tmp8], writes=[B_rtab])
                            S.op("dve", lambda e: e.reduce_sum(out=rtab[:, 1, T_:T_ + 1], in_=tmp8[:, 1, :], axis=mybir.AxisListType.X), reads=[B_tmp8], writes=[B_rtab])
                            if ft:
                                S.op("dve", lambda e: e.tensor_copy(out=selsum[:], in_=sel[:]), reads=[B_sel], writes=[B_selsum])
                            else:
                                S.op("dve", lambda e: e.tensor_tensor(out=selsum[:], in0=selsum[:], in1=sel[:], op=ALU.add), reads=[B_sel, B_selsum], writes=[B_selsum])
                            first_tile[0] = False
                            S.op("dve", lambda e: e.tensor_copy(out=rtab[:, 2, T_:T_ + 1], in_=st1[:, 5:6]), reads=[B_st], writes=[B_rtab])
                            S.op("dve", lambda e: e.tensor_copy(out=rtab[:, 3, T_:T_ + 1], in_=st1[:, 6:7]), reads=[B_st], writes=[B_rtab])
                            for k in range(8):
                                bank = 3 + k // 4
                                S.op("pe", lambda e: e.matmul(ps[bank][:, (k % 4) * 128:(k % 4 + 1) * 128], h2s[:, k, tt * 128:(tt + 1) * 128], ident_bf[:],
                                                              start=True, stop=True), sig=True, reads=[B_h2s, B_const], writes=[B_ps[bank]])
                            S.op("act", lambda e: e.activation(out=h2rows[:, T_, 0:512], in_=ps[3][:, 0:512], func=AF.Copy), reads=[B_ps[3]], writes=[B_h2rows])
                            S.op("dve", lambda e: e.tensor_copy(out=h2rows[:, T_, 512:1024], in_=ps[4][:, 0:512]), reads=[B_ps[4]], writes=[B_h2rows])
                        if not SPARSE_MOE:
                            S.dma("sp", CBd[:, c0:c0 + N], combT[:, 0:N], reads=[B_combT], writes=[B_CBd])
                if moe and SPARSE_MOE:
                    X_ = mybir.AxisListType.X
                    def dv(fn, extra_r=(), extra_w=()):
                        S.op("dve", fn, reads=[B_rt] + list(extra_r), writes=[B_rt] + list(extra_w))
                    cntv, accv, padv, endv, basev = (rt[:, j, 0:8] for j in range(5))
                    tef, bgu, bdn = rt[:, 5, 0:NTILE], rt[:, 6, 0:NTILE], rt[:, 7, 0:NTILE]
                    S.op("pe", lambda e: e.matmul(ps[2][:, 0:8], ones_f[:], selsum[:], start=True, stop=True), sig=True, reads=[B_selsum, B_const], writes=[B_ps[2]])
                    dv(lambda e: e.tensor_copy(out=cntv, in_=ps[2][:, 0:8]), extra_r=[B_ps[2]])
                    dv(lambda e: e.tensor_scalar(out=accv, in0=cntv, scalar1=0.0, scalar2=None, op0=ALU.is_gt))
                    for j in range(1, 8):
                        dv(lambda e: e.scalar_tensor_tensor(out=accv, in0=cntv, scalar=float(TS * j), in1=accv, op0=ALU.is_gt, op1=ALU.add))
                    dv(lambda e: e.tensor_scalar(out=padv, in0=accv, scalar1=float(TS), scalar2=None, op0=ALU.mult))
                    dv(lambda e: e.tensor_copy(out=endv[:, 0:1], in_=padv[:, 0:1]))
                    for ee in range(1, 8):
                        dv(lambda e: e.tensor_tensor(out=endv[:, ee:ee + 1], in0=endv[:, ee - 1:ee], in1=padv[:, ee:ee + 1], op=ALU.add))
                    dv(lambda e: e.memset(basev[:, 0:1], 0.0))
                    dv(lambda e: e.tensor_copy(out=basev[:, 1:8], in_=endv[:, 0:7]))
                    for t_ in range(NTILE):
                        dv(lambda e: e.tensor_scalar(out=tmp8[:, 0, 0:7], in0=endv[:, 0:7], scalar1=float(TS * t_), scalar2=None, op0=ALU.is_le), extra_w=[B_tmp8])
                        dv(lambda e: e.reduce_sum(out=tef[:, t_:t_ + 1], in_=tmp8[:, 0, 0:7], axis=X_), extra_r=[B_tmp8])
                    for q in range(2):
                        mks_ = mk1s if q == 0 else mk2s
                        sf_ = rt[:, q, 8:8 + 24]
                        for ee in range(8):
                            S.op("dve", lambda e: e.scalar_tensor_tensor(out=rtab[:, q, :], in0=mks_[:, :, ee], scalar=basev[:, ee:ee + 1], in1=rtab[:, q, :],
                                                                         op0=ALU.mult, op1=ALU.add), reads=[B_mks, B_rt, B_rtab], writes=[B_rtab])
                        S.op("dve", lambda e: e.tensor_copy(out=sloti[:, q, :], in_=rtab[:, q, :]), reads=[B_rtab], writes=[B_sloti])
                    io = vecs[:, IOTA_COL:IOTA_COL + 1]
                    dv(lambda e: e.tensor_scalar(out=bgu, in0=tef, scalar1=2048.0, scalar2=io, op0=ALU.mult, op1=ALU.add), extra_r=[B_vecs])
                    dv(lambda e: e.tensor_scalar(out=bdn, in0=tef, scalar1=float(DFF), scalar2=io, op0=ALU.mult, op1=ALU.add), extra_r=[B_vecs])
                    skp = rt[:, 4, 8:8 + NTILE]
                    dv(lambda e: e.memset(skp[:, 0:1], 0.0))
                    dv(lambda e: e.tensor_tensor(out=skp[:, 1:NTILE], in0=tef[:, 1:NTILE], in1=tef[:, 0:NTILE - 1], op=ALU.is_equal))
                    dv(lambda e: e.scalar_tensor_tensor(out=bgu, in0=skp, scalar=1.0e6, in1=bgu, op0=ALU.mult, op1=ALU.add))
                    dv(lambda e: e.scalar_tensor_tensor(out=bdn, in0=skp, scalar=1.0e6, in1=bdn, op0=ALU.mult, op1=ALU.add))
                    for hk in range(16):
                        S.op("dve", lambda e: e.tensor_scalar(out=widx_gu[:, :, hk], in0=bgu, scalar1=float((hk // 8) * 1024 + (hk % 8) * 128), scalar2=None, op0=ALU.add),
                             reads=[B_rt], writes=[B_widx])
                    for f_ in range(22):
                        S.op("dve", lambda e: e.tensor_scalar(out=widx_d[:, :, f_], in0=bdn, scalar1=float(f_ * 128), scalar2=None, op0=ALU.add),
                             reads=[B_rt], writes=[B_widx])
                    if dbg:
                        d_rt = nc.dram_tensor("dbg_rt", [128, 8, 32], F32, kind="ExternalOutput").ap()
                        B_drt = S.buf("dbg_rt", True)
                        S.dma("sp", d_rt, rt[:], reads=[B_rt], writes=[B_drt])
                    for T_ in range(32):
                        for q in range(2):
                            S.dma_fn("pool", lambda e: e.indirect_dma_start(out=XS[:, :], out_offset=bass.IndirectOffsetOnAxis(ap=sloti[:, q, T_:T_ + 1], axis=0),
                                                                            in_=h2rows[:, T_, :], in_offset=None),
                                     reads=[B_h2rows, B_sloti], writes=[B_XS])
                S.barrier()
                S.stop(f"{i}:C1")
            if moe and SPARSE_MOE:
                with ExitStack() as pc2:
                    NH = 11
                    wg = [sb(pc2, f"wg{b}", [128, 8, NH * 128], BF16) for b in range(2)]
                    wu = [sb(pc2, f"wu{b}", [128, 8, NH * 128], BF16) for b in range(2)]
                    wd = [sb(pc2, f"wd{b}", [128, NH, 1024], BF16) for b in range(2)]
                    B_w = [S.buf(f"wffn{b}", True) for b in range(2)]
                    xs = sb(pc2, "xs", [128, 4, 1024], BF16); B_xs_ = S.buf("xs", True)
                    xT = sb(pc2, "xT", [128, 8, 512], BF16); B_xT = S.buf("xT")
                    act = sb(pc2, "act", [128, NH, 512], BF16); B_act = S.buf("act")
                    sg = [sb(pc2, f"sg{b}", [128, 512], F32) for b in range(2)]; B_sg = [S.buf(f"sg{b}") for b in range(2)]
                    yacc = sb(pc2, "yacc", [128, 4, 1024], F32); B_yacc = S.buf("yacc")
                    bnd_reg = pc2.enter_context(nc.gpsimd.register("wbound"))
                    nc.gpsimd.reg_mov(bnd_reg, NE * DFF - 1)

                    def load_xs(t_):
                        S.dma("sp", xs[:], XS[t_ * TS:(t_ + 1) * TS, :].rearrange("(a s) d -> s a d", s=128), reads=[B_XS], writes=[B_xs_])
                    load_xs(0)
                    npass = 0
                    for t_ in range(NTILE):
                        for half in range(2):
                            b = npass % 2
                            for k in range(8):
                                S.dma_fn("pool", lambda e: e.indirect_dma_start(out=wg[b][:, k, :], out_offset=None, in_=WgH[:, :],
                                                                                in_offset=bass.IndirectOffsetOnAxis(ap=widx_gu[:, t_, half * 8 + k:half * 8 + k + 1], axis=0),
                                                                                bounds_check=bnd_reg, oob_is_err=False),
                                         reads=[B_widx], writes=[B_w[b]])
                                S.dma_fn("pool", lambda e: e.indirect_dma_start(out=wu[b][:, k, :], out_offset=None, in_=WuH[:, :],
                                                                                in_offset=bass.IndirectOffsetOnAxis(ap=widx_gu[:, t_, half * 8 + k:half * 8 + k + 1], axis=0),
                                                                                bounds_check=bnd_reg, oob_is_err=False),
                                         reads=[B_widx], writes=[B_w[b]])
                            for f in range(NH):
                                S.dma_fn("pool", lambda e: e.indirect_dma_start(out=wd[b][:, f, :], out_offset=None, in_=WdF[:, :],
                                                                                in_offset=bass.IndirectOffsetOnAxis(ap=widx_d[:, t_, half * NH + f:half * NH + f + 1], axis=0),
                                                                                bounds_check=bnd_reg, oob_is_err=False),
                                         reads=[B_widx], writes=[B_w[b]])
                            if half == 0:
                                for k in range(8):
                                    bank = 4 + (k % 2)
                                    for a in range(4):
                                        S.op("pe", lambda e: e.matmul(ps[bank][:, a * 128:(a + 1) * 128], xs[:, a, k * 128:(k + 1) * 128], ident_bf[:], start=True, stop=True),
                                             sig=True, reads=[B_xs_, B_const], writes=[B_ps[bank]])
                                    if k % 2 == 0:
                                        S.op("act", lambda e: e.activation(out=xT[:, k, :], in_=ps[bank][:], func=AF.Copy), reads=[B_ps[bank]], writes=[B_xT])
                                    else:
                                        S.op("dve", lambda e: e.tensor_copy(out=xT[:, k, :], in_=ps[bank][:]), reads=[B_ps[bank]], writes=[B_xT])
                            else:
                                if t_ + 1 < NTILE:
                                    load_xs(t_ + 1)
                            for f in range(NH):
                                gb, ub = 0 + (f % 2), 2 + (f % 2)
                                for k in range(8):
                                    S.op("pe", lambda e: e.matmul(ps[gb][:], wg[b][:, k, f * 128:(f + 1) * 128], xT[:, k, :], start=(k == 0), stop=(k == 7)),
                                         sig=(k == 7), reads=[B_w[b], B_xT], writes=[B_ps[gb]])
                                for k in range(8):
                                    S.op("pe", lambda e: e.matmul(ps[ub][:], wu[b][:, k, f * 128:(f + 1) * 128], xT[:, k, :], start=(k == 0), stop=(k == 7)),
                                         sig=(k == 7), reads=[B_w[b], B_xT], writes=[B_ps[ub]])
                                S.op("act", lambda e: e.activation(out=sg[f % 2][:], in_=ps[gb][:], func=AF.Silu), reads=[B_ps[gb]], writes=[B_sg[f % 2]])
                                S.op("dve", lambda e: e.tensor_tensor(out=act[:, f, :], in0=ps[ub][:], in1=sg[f % 2][:], op=ALU.mult),
                                     reads=[B_ps[ub], B_sg[f % 2]], writes=[B_act])
                            cntd = 0
                            for a in range(4):
                                for dh in range(2):
                                    ob = 4 + (cntd % 4)
                                    cntd += 1
                                    for f in range(NH):
                                        S.op("pe", lambda e: e.matmul(ps[ob][:], act[:, f, a * 128:(a + 1) * 128], wd[b][:, f, dh * 512:(dh + 1) * 512],
                                                                      start=(f == 0), stop=(f == NH - 1)), sig=(f == NH - 1), reads=[B_w[b], B_act], writes=[B_ps[ob]])
                                    if half == 0:
                                        S.op("act", lambda e: e.activation(out=yacc[:, a, dh * 512:(dh + 1) * 512], in_=ps[ob][:], func=AF.Copy),
                                             reads=[B_ps[ob]], writes=[B_yacc])
                                    else:
                                        S.op("dve", lambda e: e.tensor_tensor(out=yacc[:, a, dh * 512:(dh + 1) * 512], in0=ps[ob][:], in1=yacc[:, a, dh * 512:(dh + 1) * 512], op=ALU.add),
                                             reads=[B_ps[ob], B_yacc], writes=[B_yacc])
                            if half == 1:
                                S.dma("sp", YS[t_ * TS:(t_ + 1) * TS, :].rearrange("(a s) d -> s a d", s=128), yacc[:], reads=[B_yacc], writes=[B_YS])
                            npass += 1
                    S.barrier()
                    S.stop(f"{i}:C2s")
                with ExitStack() as pg:
                    yg = [[sb(pg, f"yg{q}{b}", [128, 1024], F32) for b in range(2)] for q in range(2)]
                    B_yg = [[S.buf(f"yg{q}{b}", True) for b in range(2)] for q in range(2)]
                    fr = [sb(pg, f"fr{b}", [128, 1024], F32) for b in range(2)]; B_fr = [S.buf(f"fr{b}") for b in range(2)]
                    fTc = sb(pg, "fTc", [128, 8, 512], F32); B_fTck = [S.buf(f"fTc{k}") for k in range(8)]
                    fsq = sb(pg, "fsqg", [128, 8, 512], BF16); B_fsq = S.buf("fsqg")
                    xc3 = sb(pg, "xc3g", [128, 8, 512], F32); B_xc3 = S.buf("xc3g", True)
                    rs3 = sb(pg, "rs3g", [128, 512], F32); B_rs3 = S.buf("rs3g")
                    tm3 = sb(pg, "tm3g", [128, 512], F32); B_tm3 = S.buf("tm3g")
                    xo3 = sb(pg, "xo3g", [128, 8, 512], F32); B_xo3 = S.buf("xo3g")
                    for c in range(8):
                        S.dma("sp", xc3[:], xview(x_mid)[:, :, c * 512:(c + 1) * 512], reads=[B_xm], writes=[B_xc3])
                        for tt in range(4):
                            T_ = c * 4 + tt
                            b = T_ % 2
                            for q in range(2):
                                S.dma_fn("pool", lambda e: e.indirect_dma_start(out=yg[q][b][:, :], out_offset=None, in_=YS[:, :],
                                                                                in_offset=bass.IndirectOffsetOnAxis(ap=sloti[:, q, T_:T_ + 1], axis=0)),
                                         reads=[B_YS, B_sloti], writes=[B_yg[q][b]])
                            S.op("dve", lambda e: e.tensor_scalar(out=fr[b][:], in0=yg[0][b][:], scalar1=rtab[:, 2, T_:T_ + 1], scalar2=None, op0=ALU.mult),
                                 reads=[B_yg[0][b], B_rtab], writes=[B_fr[b]])
                            S.op("dve", lambda e: e.scalar_tensor_tensor(out=fr[b][:], in0=yg[1][b][:], scalar=rtab[:, 3, T_:T_ + 1], in1=fr[b][:], op0=ALU.mult, op1=ALU.add),
                                 reads=[B_yg[1][b], B_rtab, B_fr[b]], writes=[B_fr[b]])
                            for k in range(8):
                                S.op("pe", lambda e: e.matmul(ps[k][:, tt * 128:(tt + 1) * 128], fr[b][:, k * 128:(k + 1) * 128], ident[:], start=True, stop=True),
                                     sig=True, reads=[B_fr[b], B_const], writes=[B_ps[k]])
                        for k in range(8):
                            S.op("act", lambda e: e.activation(out=fTc[:, k, :], in_=ps[k][:], func=AF.Copy), reads=[B_ps[k]], writes=[B_fTck[k]])
                            S.op("act", lambda e: e.activation(out=fsq[:, k, :], in_=ps[k][:], func=AF.Square), reads=[B_ps[k]], writes=[B_fsq])
                        for k in range(8):
                            S.op("pe", lambda e: e.matmul(ps[0][:], ones_bf[:], fsq[:, k, :], start=(k == 0), stop=(k == 7)),
                                 sig=(k == 7), reads=[B_fsq, B_const], writes=[B_ps[0]])
                        rstd_from_ss(ps[0][:], B_ps[0], D, rs3[:], B_rs3, tm3[:], B_tm3)
                        for k in range(8):
                            S.op("dve", lambda e: e.scalar_tensor_tensor(out=fTc[:, k, :], in0=fTc[:, k, :], scalar=G_f[:, k, 0:1], in1=rs3[:],
                                                                         op0=ALU.mult, op1=ALU.mult), reads=[B_fTck[k], B_rs3, B_mod], writes=[B_fTck[k]])
                            S.op("pool", lambda e: e.tensor_tensor(out=xo3[:, k, :], in0=fTc[:, k, :], in1=xc3[:, k, :], op=ALU.add),
                                 reads=[B_fTck[k], B_xc3], writes=[B_xo3])
                        S.dma("sp", xview(x_dst)[:, :, c * 512:(c + 1) * 512], xo3[:], reads=[B_xo3], writes=[B_xd])
                    S.barrier()
                    S.stop(f"{i}:C2b")
            else:
                with ExitStack() as pc2:
                    NH = 11
                    wg = [sb(pc2, f"wg{b}", [128, 8, NH * 128], BF16) for b in range(2)]
                    wu = [sb(pc2, f"wu{b}", [128, 8, NH * 128], BF16) for b in range(2)]
                    wd = [sb(pc2, f"wd{b}", [128, NH, 1024], BF16) for b in range(2)]
                    B_w = [S.buf(f"wffn{b}", True) for b in range(2)]
                    h2c = [sb(pc2, f"h2c{b}", [128, 8, 512], BF16) for b in range(2)]
                    B_h2c = [S.buf(f"h2c{b}", True) for b in range(2)]
                    act = sb(pc2, "act", [128, NH, 512], BF16); B_act = S.buf("act")
                    sg = [sb(pc2, f"sg{b}", [128, 512], F32) for b in range(2)]; B_sg = [S.buf(f"sg{b}") for b in range(2)]
                    facc = sb(pc2, "facc", [128, 8, 512], F32); B_facc = S.buf("facc", True)
                    cbc = sb(pc2, "cbc", [128, 512], F32); B_cbc = S.buf("cbc", True)
                    otmp = [sb(pc2, f"otmp{b}", [128, 512], F32) for b in range(2)]; B_otmp = [S.buf(f"otmp{b}") for b in range(2)]
                    npass = 0
                    for ex in range(n_exp):
                        if moe:
                            Wg, Wu, Wd = W["moe_w_gate"][0, ex], W["moe_w_up"][0, ex], W["moe_w_down"][0, ex]
                        else:
                            Wg, Wu, Wd = W["ffn_w_gate"][0], W["ffn_w_up"][0], W["ffn_w_down"][0]
                        Wgv = Wg.rearrange("(k p) n -> p k n", p=128)
                        Wuv = Wu.rearrange("(k p) n -> p k n", p=128)
                        Wdv = Wd.rearrange("(f p) n -> p f n", p=128)
                        for half in range(2):
                            b = npass % 2
                            f0 = half * NH
                            for k in range(8):
                                S.dma("pool", wg[b][:, k, :], Wgv[:, k, f0 * 128:(f0 + NH) * 128], writes=[B_w[b]])
                                S.dma("pool", wu[b][:, k, :], Wuv[:, k, f0 * 128:(f0 + NH) * 128], writes=[B_w[b]])
                            for f in range(NH):
                                S.dma("pool", wd[b][:, f, :], Wdv[:, f0 + f, :], writes=[B_w[b]])
                            def load_h2c(cj):
                                c0j, Nj, _ = ffn_chunks[cj]
                                S.dma("sp", h2c[cj % 2][:, :, 0:Nj], H2T[:, :, c0j:c0j + Nj].rearrange("k p n -> p k n"), reads=[B_H2T], writes=[B_h2c[cj % 2]])
                            load_h2c(0)
                            for ci, (c0, N, isc) in enumerate(ffn_chunks):
                                hb = ci % 2
                                if ci + 1 < len(ffn_chunks):
                                    load_h2c(ci + 1)
                                if npass > 0:
                                    S.dma("sp", facc[:, :, 0:N], FT[:, :, c0:c0 + N].rearrange("k p n -> p k n"), reads=[B_FT], writes=[B_facc])
                                if moe:
                                    cb_src = bass.AP(CBd.tensor, ex * NTOK + c0, [[0, 128], [1, N]])
                                    S.dma("sp", cbc[:, 0:N], cb_src, reads=[B_CBd], writes=[B_cbc])
                                for f in range(NH):
                                    gb, ub = 0 + (f % 2), 2 + (f % 2)
                                    for k in range(8):
                                        S.op("pe", lambda e: e.matmul(ps[gb][:, 0:N], wg[b][:, k, f * 128:(f + 1) * 128], h2c[hb][:, k, 0:N],
                                                                      start=(k == 0), stop=(k == 7)), sig=(k == 7), reads=[B_w[b], B_h2c[hb]], writes=[B_ps[gb]])
                                    for k in range(8):
                                        S.op("pe", lambda e: e.matmul(ps[ub][:, 0:N], wu[b][:, k, f * 128:(f + 1) * 128], h2c[hb][:, k, 0:N],
                                                                      start=(k == 0), stop=(k == 7)), sig=(k == 7), reads=[B_w[b], B_h2c[hb]], writes=[B_ps[ub]])
                                    S.op("act", lambda e: e.activation(out=sg[f % 2][:, 0:N], in_=ps[gb][:, 0:N], func=AF.Silu), reads=[B_ps[gb]], writes=[B_sg[f % 2]])
                                    S.op("dve", lambda e: e.tensor_tensor(out=act[:, f, 0:N], in0=ps[ub][:, 0:N], in1=sg[f % 2][:, 0:N], op=ALU.mult),
                                         reads=[B_ps[ub], B_sg[f % 2]], writes=[B_act])
                                for j in range(8):
                                    ob = 4 + (j % 4)
                                    for f in range(NH):
                                        S.op("pe", lambda e: e.matmul(ps[ob][:, 0:N], wd[b][:, f, j * 128:(j + 1) * 128], act[:, f, 0:N],
                                                                      start=(f == 0), stop=(f == NH - 1)), sig=(f == NH - 1), reads=[B_w[b], B_act], writes=[B_ps[ob]])
                                    if moe:
                                        S.op("dve", lambda e: e.tensor_tensor(out=otmp[j % 2][:, 0:N], in0=ps[ob][:, 0:N], in1=cbc[:, 0:N], op=ALU.mult),
                                             reads=[B_ps[ob], B_cbc], writes=[B_otmp[j % 2]])
                                        if npass == 0:
                                            S.op("pool", lambda e: e.tensor_copy(out=facc[:, j, 0:N], in_=otmp[j % 2][:, 0:N]), reads=[B_otmp[j % 2]], writes=[B_facc])
                                        else:
                                            S.op("pool", lambda e: e.tensor_tensor(out=facc[:, j, 0:N], in0=facc[:, j, 0:N], in1=otmp[j % 2][:, 0:N], op=ALU.add),
                                                 reads=[B_otmp[j % 2], B_facc], writes=[B_facc])
                                    else:
                                        if npass == 0:
                                            S.op("act", lambda e: e.activation(out=facc[:, j, 0:N], in_=ps[ob][:, 0:N], func=AF.Copy), reads=[B_ps[ob]], writes=[B_facc])
                                        else:
                                            S.op("dve", lambda e: e.tensor_tensor(out=facc[:, j, 0:N], in0=ps[ob][:, 0:N], in1=facc[:, j, 0:N], op=ALU.add),
                                                 reads=[B_ps[ob], B_facc], writes=[B_facc])
                                S.dma("sp", FT[:, :, c0:c0 + N].rearrange("k p n -> p k n"), facc[:, :, 0:N], reads=[B_facc], writes=[B_FT])
                            npass += 1
                    S.barrier()
                    S.stop(f"{i}:C2")
            with ExitStack() as pc3:
                fc = sb(pc3, "fc", [128, 8, 512], F32); B_fc = S.buf("fc", True)
                fsq = sb(pc3, "fsq", [128, 8, 512], BF16); B_fsq = S.buf("fsq")
                xc3 = sb(pc3, "xc3", [128, 8, 512], F32); B_xc3 = S.buf("xc3", True)
                rs3 = sb(pc3, "rs3", [128, 512], F32); B_rs3 = S.buf("rs3")
                tm3 = sb(pc3, "tm3", [128, 512], F32); B_tm3 = S.buf("tm3")
                xo3 = sb(pc3, "xo3", [128, 8, 512], F32); B_xo3 = S.buf("xo3")
                nxt_mod = (i + 1 < n_layers)
                if nxt_mod:
                    wm_n, B_wm_n = mod_alloc(pc3)
                    mod_gs = list(range(12))
                for ci, (c0, N, isc) in enumerate([] if (moe and SPARSE_MOE) else ffn_chunks):
                    if nxt_mod and mod_gs:
                        mod_group(i + 1, mod_gs.pop(0), wm_n, B_wm_n)
                        if ci % 2 == 1 and mod_gs:
                            mod_group(i + 1, mod_gs.pop(0), wm_n, B_wm_n)
                    s = 1 if isc else 0
                    cs = 0 if isc else c0
                    S.dma("sp", fc[:, :, 0:N], FT[:, :, c0:c0 + N].rearrange("k p n -> p k n"), reads=[B_FT], writes=[B_fc])
                    S.dma("sp", xc3[:, :, 0:N], xview(y_mid if isc else x_mid)[:, :, cs:cs + N], reads=[B_ym if isc else B_xm], writes=[B_xc3])
                    for k in range(8):
                        S.op("act", lambda e: e.activation(out=fsq[:, k, 0:N], in_=fc[:, k, 0:N], func=AF.Square), reads=[B_fc], writes=[B_fsq])
                    for k in range(8):
                        S.op("pe", lambda e: e.matmul(ps[0][:, 0:N], ones_bf[:], fsq[:, k, 0:N], start=(k == 0), stop=(k == 7)),
                             sig=(k == 7), reads=[B_fsq, B_const], writes=[B_ps[0]])
                    rstd_from_ss(ps[0][:, 0:N], B_ps[0], D, rs3[:, 0:N], B_rs3, tm3[:, 0:N], B_tm3)
                    for k in range(8):
                        S.op("dve", lambda e: e.scalar_tensor_tensor(out=fc[:, k, 0:N], in0=fc[:, k, 0:N], scalar=G_f[:, k, s:s + 1], in1=rs3[:, 0:N],
                                                                     op0=ALU.mult, op1=ALU.mult), reads=[B_fc, B_rs3, B_mod], writes=[B_fc])
                        S.op("pool", lambda e: e.tensor_tensor(out=xo3[:, k, 0:N], in0=fc[:, k, 0:N], in1=xc3[:, k, 0:N], op=ALU.add),
                             reads=[B_fc, B_xc3], writes=[B_xo3])
                    S.dma("sp", xview(y_dst if isc else x_dst)[:, :, cs:cs + N], xo3[:, :, 0:N], reads=[B_xo3], writes=[B_yd if isc else B_xd])
                if nxt_mod:
                    while mod_gs:
                        mod_group(i + 1, mod_gs.pop(0), wm_n, B_wm_n)
                    mod_finish(i + 1)
                S.barrier()
            x_src, B_xs = x_dst, B_xd
            y_src, B_ys = y_dst, B_yd
        S.dead = False
        if dbg:
            d1 = nc.dram_tensor("dbg_sloti", [128, 2, 32], I32, kind="ExternalOutput").ap()
            d2 = nc.dram_tensor("dbg_rtab", [128, 4, 32], F32, kind="ExternalOutput").ap()
            d3 = nc.dram_tensor("dbg_widx_gu", [128, NTILE, 16], I32, kind="ExternalOutput").ap()
            d4 = nc.dram_tensor("dbg_widx_d", [128, NTILE, 22], I32, kind="ExternalOutput").ap()
            B_dd = S.buf("dbg_dd", True)
            S.dma("sp", d1, sloti[:], reads=[B_sloti], writes=[B_dd])
            S.dma("sp", d2, rtab[:], reads=[B_rtab], writes=[B_dd])
            S.dma("sp", d3, widx_gu[:], reads=[B_widx], writes=[B_dd])
            S.dma("sp", d4, widx_d[:], reads=[B_widx], writes=[B_dd])
        if n_layers < DEPTH or stop_after is not None:
            with ExitStack() as pd:
                t = sb(pd, "dcp", [128, 8, 512], F32); B_t = S.buf("dcp", True)
                for c in range(8):
                    S.dma("sp", t[:], xview(x_src)[:, :, c * 512:(c + 1) * 512], reads=[B_xs], writes=[B_t])
                    S.dma("sp", xview(outT)[:, :, c * 512:(c + 1) * 512], t[:], reads=[B_t], writes=[B_out])
        S.barrier()
        print(f"[kernel] instructions={S.n_inst} waits={S.n_wait} dsems={len(S.dsems)}")
    return nc


def _shared_inputs(inp):
    cos2, sin2, pm, ident, invt, triu = _const_tables()
    shared = {"cos2": cos2, "sin2": sin2, "pm": pm, "ident": ident, "invt": invt, "triu": triu}
    for nm in WEIGHT_NAMES:
        shared[nm] = np.ascontiguousarray(inp[nm], dtype=np.float32)
    if SPARSE_MOE:
        for nm in ("moe_w_gate", "moe_w_up"):
            w = shared.pop(nm)[0]
            shared[nm + "_h"] = np.ascontiguousarray(w.reshape(NE, D, 2, DFF // 2).transpose(0, 2, 1, 3)).reshape(NE * 2 * D, DFF // 2)
        shared["moe_w_down_f"] = np.ascontiguousarray(shared.pop("moe_w_down")[0]).reshape(NE * DFF, D)
    return shared


_PROGRAM = None


def kernel(**inputs):
    global _PROGRAM
    inp = {k: np.asarray(v) for k, v in inputs.items()}
    n = 8
    shared = _shared_inputs(inp)
    in_maps = []
    for b in range(n):
        m = dict(shared)
        m["xT"] = np.ascontiguousarray(inp["x"][b].T, dtype=np.float32)
        m["ctxT"] = np.ascontiguousarray(inp["ctx"][b].T, dtype=np.float32)
        m["vecs"] = _pack_vecs(inp, b)
        in_maps.append(m)
    if _PROGRAM is None:
        _PROGRAM = build_program()
    res = run_bass_kernel_spmd(_PROGRAM, in_maps, core_ids=list(range(n)))
    out = np.stack([np.ascontiguousarray(res.results[b]["outT"].T) for b in range(n)], axis=0)
    return out.astype(np.float32)
```
